# Optimizing a Trainium2 kernel written in Bass

```python
import jax, jax.numpy as jnp
from jax import lax
import numpy as np

D_MODEL = 1024
BATCH = 8
SEQ = 4096
DEPTH = 1

GRID_W = 64
CTX_LEN = 256
EPS = 1e-6
ATTN_HEADS = 8
ATTN_KV_HEADS = 2
ATTN_HEAD_DIM = 64
ATTN_WIDTH = ATTN_HEADS * ATTN_HEAD_DIM
KV_WIDTH = ATTN_KV_HEADS * ATTN_HEAD_DIM
Q_BLOCK = 128
ROPE_THETA = 10000.0
HGRN_HEADS = 4
HGRN_HEAD_DIM = 128
HGRN_WIDTH = HGRN_HEADS * HGRN_HEAD_DIM
HGRN_CHUNK = 64
MIX_WIDTH = ATTN_WIDTH + HGRN_WIDTH
IN_PROJ_WIDTH = ATTN_WIDTH + 2 * KV_WIDTH + 5 * HGRN_WIDTH
N_GROUPS = 4
EXPERTS_PER_GROUP = 8
N_EXPERTS = N_GROUPS * EXPERTS_PER_GROUP
TOP_K = 2
EXPERT_FF = 512
MOE_BLOCK = 128

kernel_name = "hymba_style_gqa_hgrn2_hmoe_dit_layer"


def rms_norm(x, g):
    xf = x.astype(jnp.float32)
    y = xf * lax.rsqrt(jnp.mean(xf * xf, axis=-1, keepdims=True) + EPS)
    return (y * g.astype(jnp.float32)).astype(x.dtype)


def head_rms_norm(x, g, n_heads):
    shp = x.shape
    xf = x.astype(jnp.float32).reshape(shp[:-1] + (n_heads, shp[-1] // n_heads))
    xf = xf * lax.rsqrt(jnp.mean(xf * xf, axis=-1, keepdims=True) + EPS)
    return (xf.reshape(shp) * g.astype(jnp.float32)).astype(x.dtype)


def modulate(x, g, shift, scale):
    return rms_norm(x, g) * (1 + scale) + shift


def adaln(cond, w_ada, b_ada):
    m = jax.nn.silu(cond) @ w_ada + b_ada
    return jnp.split(m, 6, axis=-1)


def split_proj(p):
    points = []
    acc = 0
    for w in (ATTN_WIDTH, KV_WIDTH, KV_WIDTH, HGRN_WIDTH, HGRN_WIDTH, HGRN_WIDTH, HGRN_WIDTH):
        acc += w
        points.append(acc)
    return jnp.split(p, points, axis=-1)


def axial_rope_tables(n_tokens):
    rows = n_tokens // GRID_W
    row = jnp.repeat(jnp.arange(rows), GRID_W).astype(jnp.float32)
    col = jnp.tile(jnp.arange(GRID_W), rows).astype(jnp.float32)
    half = ATTN_HEAD_DIM // 2
    freqs = ROPE_THETA ** (-jnp.arange(0, half, 2, dtype=jnp.float32) / half)
    ang = jnp.stack([row[:, None] * freqs, col[:, None] * freqs], axis=1)
    return jnp.cos(ang), jnp.sin(ang)


def apply_rope(x, cos, sin):
    B, S, H, dh = x.shape
    xr = x.astype(jnp.float32).reshape(B, S, H, 2, dh // 4, 2)
    x1, x2 = xr[..., 0], xr[..., 1]
    c = cos[None, :, None]
    s = sin[None, :, None]
    out = jnp.stack([x1 * c - x2 * s, x1 * s + x2 * c], axis=-1)
    return out.reshape(B, S, H, dh).astype(x.dtype)


def attention_group(q_l, k_l, v_l, q_c, k_c, v_c, q_norm_g, k_norm_g, with_ctx):
    B, S, _ = q_l.shape
    L = q_c.shape[1]
    G = ATTN_HEADS // ATTN_KV_HEADS

    def heads(t, h):
        return t.reshape(t.shape[0], t.shape[1], h, ATTN_HEAD_DIM)

    cos, sin = axial_rope_tables(S)
    ql = apply_rope(rms_norm(heads(q_l, ATTN_HEADS), q_norm_g), cos, sin)
    kl = apply_rope(rms_norm(heads(k_l, ATTN_KV_HEADS), k_norm_g), cos, sin)
    vl = heads(v_l, ATTN_KV_HEADS)
    kc = rms_norm(heads(k_c, ATTN_KV_HEADS), k_norm_g)
    vc = heads(v_c, ATTN_KV_HEADS)
    scale = ATTN_HEAD_DIM ** -0.5
    k_all = jnp.concatenate([kc, kl], axis=1)
    v_all = jnp.concatenate([vc, vl], axis=1)

    def attend(q, k, v):
        s = jnp.einsum('bqkgd,bskd->bkgqs', q, k).astype(jnp.float32) * scale
        p = jax.nn.softmax(s, axis=-1).astype(v.dtype)
        return jnp.einsum('bkgqs,bskd->bqkgd', p, v)

    nb = S // Q_BLOCK
    qb = jnp.moveaxis(ql.reshape(B, nb, Q_BLOCK, ATTN_KV_HEADS, G, ATTN_HEAD_DIM), 1, 0)
    ob = lax.map(lambda q: attend(q, k_all, v_all), qb)
    o_l = jnp.moveaxis(ob, 0, 1).reshape(B, S, ATTN_WIDTH)
    o_c = None
    if with_ctx:
        qc = rms_norm(heads(q_c, ATTN_HEADS), q_norm_g)
        qc = qc.reshape(B, L, ATTN_KV_HEADS, G, ATTN_HEAD_DIM)
        o_c = attend(qc, kc, vc).reshape(B, L, ATTN_WIDTH)
    return o_l, o_c


def gla_scan(q, k, v, logf, s0):
    B, L, H, dk = q.shape
    dv = v.shape[-1]
    n = L // HGRN_CHUNK

    def chunks(t):
        return jnp.moveaxis(t.astype(jnp.float32).reshape(B, n, HGRN_CHUNK, H, t.shape[-1]), 1, 0)

    causal = jnp.tril(jnp.ones((HGRN_CHUNK, HGRN_CHUNK), dtype=bool))[None, :, :, None, None]

    def step(S, inp):
        qc, kc, vc, gc = inp
        b = jnp.cumsum(gc, axis=1)
        inter = jnp.einsum('bthd,bhdv->bthv', qc * jnp.exp(b), S)
        decay = jnp.exp(jnp.where(causal, b[:, :, None] - b[:, None, :], -jnp.inf))
        A = jnp.einsum('bthd,bshd,btshd->bhts', qc, kc, decay)
        intra = jnp.einsum('bhts,bshv->bthv', A, vc)
        b_end = b[:, -1]
        S_new = jnp.exp(b_end)[..., None] * S + jnp.einsum(
            'bshd,bshv->bhdv', kc * jnp.exp(b_end[:, None] - b), vc)
        return S_new, inter + intra

    S, o = lax.scan(step, s0, (chunks(q), chunks(k), chunks(v), chunks(logf)))
    return S, jnp.moveaxis(o, 0, 1).reshape(B, L, H, dv)


def hgrn2_group(lat, ctx, lb):
    def heads(t):
        return t.reshape(t.shape[0], t.shape[1], HGRN_HEADS, HGRN_HEAD_DIM)

    def prep(q, ff, fb, i):
        q = jax.nn.silu(q.astype(jnp.float32)) * (HGRN_HEAD_DIM ** -0.5)

        def gate(fr, lbd):
            f = lbd + (1 - lbd) * jax.nn.sigmoid(fr.astype(jnp.float32))
            return heads(1 - f), heads(jnp.log(f))

        return heads(q), heads(i), gate(ff, lb[0]), gate(fb, lb[1])

    qc, ic, (kcf, gcf), (kcb, gcb) = prep(*ctx)
    ql, il, (klf, glf), (klb, glb) = prep(*lat)
    B = ql.shape[0]
    s0 = jnp.zeros((B, HGRN_HEADS, HGRN_HEAD_DIM, HGRN_HEAD_DIM), jnp.float32)

    def flip(t):
        return jnp.flip(t, axis=1)

    s_cf, o_cf = gla_scan(qc, kcf, ic, gcf, s0)
    s_cb, o_cb = gla_scan(flip(qc), flip(kcb), flip(ic), flip(gcb), s0)
    _, o_lf = gla_scan(ql, klf, il, glf, s_cf)
    _, o_lb = gla_scan(flip(ql), flip(klb), flip(il), flip(glb), s_cb)
    o_l = o_lf + flip(o_lb)
    o_c = o_cf + flip(o_cb)
    return (o_l.reshape(o_l.shape[0], o_l.shape[1], HGRN_WIDTH),
            o_c.reshape(o_c.shape[0], o_c.shape[1], HGRN_WIDTH))


def mixing(h, hc, w_in, q_norm_g, k_norm_g, attn_out_g, lb, hgrn_out_g, w_out, with_ctx):
    qa, ka, va, qr, ffr, fbr, ir, gr = split_proj(h @ w_in)
    qac, kac, vac, qrc, ffrc, fbrc, irc, grc = split_proj(hc @ w_in)
    oa, oac = attention_group(qa, ka, va, qac, kac, vac, q_norm_g, k_norm_g, with_ctx)
    orl, orc = hgrn2_group((qr, ffr, fbr, ir), (qrc, ffrc, fbrc, irc), lb)
    oa = head_rms_norm(oa, attn_out_g, ATTN_HEADS)
    orl = (head_rms_norm(orl, hgrn_out_g, HGRN_HEADS) * jax.nn.silu(gr)).astype(h.dtype)
    mix_l = jnp.concatenate([oa, orl], axis=-1) @ w_out
    mix_c = None
    if with_ctx:
        oac = head_rms_norm(oac, attn_out_g, ATTN_HEADS)
        orc = (head_rms_norm(orc, hgrn_out_g, HGRN_HEADS) * jax.nn.silu(grc)).astype(hc.dtype)
        mix_c = jnp.concatenate([oac, orc], axis=-1) @ w_out
    return mix_l, mix_c


def moe_ffn(h, w_router_grp, b_router_grp, w_router_exp, b_router_exp, w_exp_gate, w_exp_up, w_exp_down):
    N, D = h.shape
    pg = jax.nn.softmax((h @ w_router_grp).astype(jnp.float32) + b_router_grp, axis=-1)
    pg_top, g_sel = lax.top_k(pg, 1)
    le = ((h @ w_router_exp).astype(jnp.float32) + b_router_exp).reshape(N, N_GROUPS, EXPERTS_PER_GROUP)
    le = jnp.take_along_axis(le, g_sel[:, :, None], axis=1)[:, 0]
    pe = jax.nn.softmax(le, axis=-1)
    pe_top, e_loc = lax.top_k(pe, TOP_K)
    w = pg_top * pe_top / jnp.sum(pe_top, axis=-1, keepdims=True)
    e_id = g_sel * EXPERTS_PER_GROUP + e_loc

    A = N * TOP_K
    e_flat = e_id.reshape(-1)
    tok = jnp.repeat(jnp.arange(N, dtype=jnp.int32), TOP_K)
    w_flat = w.reshape(-1)
    order = jnp.argsort(e_flat)
    e_s, tok_s, w_s = e_flat[order], tok[order], w_flat[order]
    counts = jnp.bincount(e_flat, length=N_EXPERTS)
    padded = (counts + MOE_BLOCK - 1) // MOE_BLOCK * MOE_BLOCK
    start = jnp.cumsum(counts) - counts
    pend = jnp.cumsum(padded)
    pstart = pend - padded
    dest = pstart[e_s] + (jnp.arange(A, dtype=jnp.int32) - start[e_s])
    cap = -(-A // MOE_BLOCK) * MOE_BLOCK + N_EXPERTS * MOE_BLOCK
    n_blk = cap // MOE_BLOCK
    slot_tok = jnp.zeros((cap,), jnp.int32).at[dest].set(tok_s)
    slot_w = jnp.zeros((cap,), jnp.float32).at[dest].set(w_s)
    blk_start = jnp.arange(n_blk, dtype=jnp.int32) * MOE_BLOCK
    blk_e = jnp.minimum(jnp.sum(blk_start[:, None] >= pend[None, :], axis=1), N_EXPERTS - 1)
    xs = h[slot_tok].reshape(n_blk, MOE_BLOCK, D)

    def expert_block(args):
        xb, e = args
        a = jax.nn.silu(xb @ w_exp_gate[e]) * (xb @ w_exp_up[e])
        return a @ w_exp_down[e]

    ys = lax.map(expert_block, (xs, blk_e)).reshape(cap, D)
    out = jnp.zeros((N, D), jnp.float32).at[slot_tok].add(ys.astype(jnp.float32) * slot_w[:, None])
    return out.astype(h.dtype)


def trunk_layer(x, xc, c, c_ctx, w_ada, b_ada, norm1_g, norm2_g, w_in, q_norm_g, k_norm_g,
                attn_out_g, lb, hgrn_out_g, w_out, w_router_grp, b_router_grp, w_router_exp,
                b_router_exp, w_exp_gate, w_exp_up, w_exp_down, with_ctx):
    B, S, D = x.shape
    sh1, sc1, gt1, sh2, sc2, gt2 = adaln(c, w_ada, b_ada)
    csh1, csc1, cgt1, csh2, csc2, cgt2 = adaln(c_ctx, w_ada, b_ada)
    h = modulate(x, norm1_g, sh1[:, None], sc1[:, None])
    hc = modulate(xc, norm1_g, csh1, csc1)
    mix_l, mix_c = mixing(h, hc, w_in, q_norm_g, k_norm_g, attn_out_g, lb, hgrn_out_g, w_out, with_ctx)
    moe_args = (w_router_grp, b_router_grp, w_router_exp, b_router_exp, w_exp_gate, w_exp_up, w_exp_down)
    x = x + gt1[:, None] * mix_l
    h2 = modulate(x, norm2_g, sh2[:, None], sc2[:, None])
    x = x + gt2[:, None] * moe_ffn(h2.reshape(B * S, D), *moe_args).reshape(B, S, D)
    if with_ctx:
        xc = xc + cgt1 * mix_c
        h2c = modulate(xc, norm2_g, csh2, csc2)
        xc = xc + cgt2 * moe_ffn(h2c.reshape(-1, D), *moe_args).reshape(xc.shape)
    return x, xc


def setup_inputs(seed: int = 0) -> dict:
    key = jax.random.key(seed)
    ks = jax.random.split(key, 22)
    f32 = jnp.float32
    D = D_MODEL

    def nrm(k, shape, s):
        return jax.random.normal(k, shape, f32) * s

    return {
        "x": nrm(ks[0], (BATCH, SEQ, D), 1.0),
        "c": nrm(ks[1], (BATCH, D), 1.0),
        "ctx": nrm(ks[2], (BATCH, CTX_LEN, D), 1.0),
        "c_ctx": nrm(ks[3], (D,), 1.0),
        "w_ada": nrm(ks[4], (DEPTH, D, 6 * D), 0.5 * D ** -0.5),
        "b_ada": nrm(ks[5], (DEPTH, 6 * D), 0.01),
        "norm1_g": 1.0 + nrm(ks[6], (DEPTH, D), 0.1),
        "norm2_g": 1.0 + nrm(ks[7], (DEPTH, D), 0.1),
        "w_in": nrm(ks[8], (DEPTH, D, IN_PROJ_WIDTH), D ** -0.5),
        "q_norm_g": 1.0 + nrm(ks[9], (DEPTH, ATTN_HEAD_DIM), 0.1),
        "k_norm_g": 1.0 + nrm(ks[10], (DEPTH, ATTN_HEAD_DIM), 0.1),
        "attn_out_g": 1.0 + nrm(ks[11], (DEPTH, ATTN_WIDTH), 0.1),
        "hgrn_lb": nrm(ks[12], (2, DEPTH + 1, HGRN_WIDTH), 0.5),
        "hgrn_out_g": 1.0 + nrm(ks[13], (DEPTH, HGRN_WIDTH), 0.1),
        "w_out": nrm(ks[14], (DEPTH, MIX_WIDTH, D), MIX_WIDTH ** -0.5),
        "w_router_grp": nrm(ks[15], (DEPTH, D, N_GROUPS), D ** -0.5),
        "b_router_grp": nrm(ks[16], (DEPTH, N_GROUPS), 0.01),
        "w_router_exp": nrm(ks[17], (DEPTH, D, N_EXPERTS), D ** -0.5),
        "b_router_exp": nrm(ks[18], (DEPTH, N_EXPERTS), 0.01),
        "w_exp_gate": nrm(ks[19], (DEPTH, N_EXPERTS, D, EXPERT_FF), D ** -0.5),
        "w_exp_up": nrm(ks[20], (DEPTH, N_EXPERTS, D, EXPERT_FF), D ** -0.5),
        "w_exp_down": nrm(ks[21], (DEPTH, N_EXPERTS, EXPERT_FF, D), EXPERT_FF ** -0.5),
    }


def reference(x, c, ctx, c_ctx, w_ada, b_ada, norm1_g, norm2_g, w_in, q_norm_g, k_norm_g,
              attn_out_g, hgrn_lb, hgrn_out_g, w_out, w_router_grp, b_router_grp, w_router_exp,
              b_router_exp, w_exp_gate, w_exp_up, w_exp_down):
    lb_all = jnp.cumsum(jax.nn.softmax(hgrn_lb.astype(jnp.float32), axis=1), axis=1)
    xc = ctx
    for layer in range(DEPTH):
        x, xc = trunk_layer(
            x, xc, c, c_ctx, w_ada[layer], b_ada[layer], norm1_g[layer], norm2_g[layer],
            w_in[layer], q_norm_g[layer], k_norm_g[layer], attn_out_g[layer], lb_all[:, layer],
            hgrn_out_g[layer], w_out[layer], w_router_grp[layer], b_router_grp[layer],
            w_router_exp[layer], b_router_exp[layer], w_exp_gate[layer], w_exp_up[layer],
            w_exp_down[layer], with_ctx=layer < DEPTH - 1)
    return x
```

```python
import numpy as np
from contextlib import ExitStack
import concourse.bass as bass
import concourse.mybir as mybir
from concourse.bass_utils import run_bass_kernel_spmd

F32 = mybir.dt.float32
BF16 = mybir.dt.bfloat16
I32 = mybir.dt.int32
AF = mybir.ActivationFunctionType
ALU = mybir.AluOpType
AX = mybir.AxisListType

NCORES = 8
LIM = [99, 0]
DM = 1024
SEQ = 4096
CTX = 256
TT = SEQ + CTX
NT = TT // 128
NLT = SEQ // 128
EPS = 1e-6
NBLK = 96
CAP = NBLK * 128
OUT_ROWS = SEQ + 128


class Sched:
    def __init__(self, nc, stack):
        self.nc = nc
        self.stack = stack
        self.eng = {"pe": nc.tensor, "act": nc.scalar, "dve": nc.vector, "pool": nc.gpsimd, "sp": nc.sync}
        self.sem = {e: stack.enter_context(nc.semaphore("s_" + e)) for e in self.eng}
        self.cnt = {e: 0 for e in self.eng}
        self.seen = {e: {} for e in self.eng}
        self.dsem = {}
        self.last_w = {}
        self.readers = {}
        self.nins = 0
        self.defer = None
        self.atom = None

    def _semof(self, key):
        if isinstance(key, tuple):
            return self.dsem[key[1]][0]
        return self.sem[key]

    def _deps(self, e, R, W):
        deps = {}

        def need(k, v):
            if v > deps.get(k, 0):
                deps[k] = v
        for b in R:
            w = self.last_w.get(b)
            if w:
                need(*w)
        for b in W:
            w = self.last_w.get(b)
            if w and (w[0] != e or e != "pe"):
                need(*w)
            for r in self.readers.get(b, ()):
                if r[0] != e or e != "pe":
                    need(*r)
        for k, v in deps.items():
            if self.seen[e].get(k, 0) >= v:
                continue
            if k == e:
                if v > self.cnt[e]:
                    continue
            self.eng[e].wait_ge(self._semof(k), v)
            self.seen[e][k] = v

    def op(self, e, fn, R=(), W=(), signal=True):
        if self.defer is not None:
            name, a, k = self._record(fn)
            R, W = list(R), list(W)
            tgt_list = self.atom if self.atom is not None else self.defer
            tgt_list.append(lambda: self.op(e, lambda eng: getattr(eng, name)(*a, **k), R, W, True))
            return None
        self._deps(e, R, W)
        ins = fn(self.eng[e])
        self.nins += 1
        tgt = self.cnt[e] + 1
        if signal:
            ins.then_inc(self.sem[e], 1)
            self.cnt[e] = tgt
        for b in R:
            self.readers.setdefault(b, []).append((e, tgt))
        for b in W:
            self.last_w[b] = (e, tgt)
            self.readers[b] = []
        return ins

    @staticmethod
    def _record(fn):
        class _Rec:
            def __getattr__(self, name):
                def f(*a, **k):
                    self.call = (name, a, k)
                return f
        r = _Rec()
        fn(r)
        return r.call

    def atomic_begin(self):
        if self.defer is not None:
            self.atom = []

    def atomic_end(self):
        if self.defer is not None and self.atom is not None:
            lst, self.atom = self.atom, None
            self.defer.append(lambda: [t() for t in lst])

    def flush(self, pend, n=None):
        n = len(pend) if n is None else min(n, len(pend))
        for _ in range(n):
            pend.pop(0)()

    def dma(self, q, fn, skey, R=(), W=()):
        if self.defer is not None:
            name, a, k = self._record(fn)
            R, W = list(R), list(W)
            self.defer.append(lambda: self.dma(q, lambda eng: getattr(eng, name)(*a, **k), skey, R, W))
            return None
        if skey not in self.dsem:
            self.dsem[skey] = [self.stack.enter_context(self.nc.semaphore("d_" + str(skey))), 0]
        ds = self.dsem[skey]
        key = ("d", skey)
        if ds[1] > 0 and self.seen[q].get(key, 0) < ds[1]:
            self.eng[q].wait_ge(ds[0], ds[1])
            self.seen[q][key] = ds[1]
        self._deps(q, R, W)
        ins = fn(self.eng[q])
        self.nins += 1
        ds[1] += 16
        ins.then_inc(ds[0], 16)
        for b in R:
            self.readers.setdefault(b, []).append((key, ds[1]))
        for b in W:
            self.last_w[b] = (key, ds[1])
            self.readers[b] = []
        return ins

    def barrier(self):
        for e in self.eng:
            for o in self.eng:
                if o != e and self.cnt[o] > self.seen[e].get(o, 0):
                    self.eng[e].wait_ge(self.sem[o], self.cnt[o])
                    self.seen[e][o] = self.cnt[o]
            for sk, (s, c) in self.dsem.items():
                k = ("d", sk)
                if c > self.seen[e].get(k, 0):
                    self.eng[e].wait_ge(s, c)
                    self.seen[e][k] = c
        self.last_w = {}
        self.readers = {}


def build(stage=99, dbg=False):
    nc = bass.Bass("TRN2", target_bir_lowering=False)

    def DI(name, shape, dt=F32):
        return nc.dram_tensor(name, shape, dt, kind="ExternalInput").ap()

    x_d = DI("x", [SEQ, DM])
    ctx_d = DI("ctx", [CTX, DM])
    cvec_d = DI("cvec", [128, 16])
    wada_d = DI("w_ada", [DM, 6 * DM])
    bada_d = DI("b_ada", [1, 6 * DM])
    n1g_d = DI("n1g", [1, DM])
    n2g_d = DI("n2g", [1, DM])
    win_d = DI("w_in", [DM, 3328])
    qkg_d = DI("qkg", [1, 640])
    aog_d = DI("aog", [128, 8])
    lbt_d = DI("lbt", [128, 16])
    wout_d = DI("w_out", [DM, DM])
    wr_d = DI("wr", [DM, 36])
    br_d = DI("br", [1, 36])
    weg_d = DI("weg", [4096, 8, 512])
    weu_d = DI("weu", [4096, 8, 512])
    wed_d = DI("wed", [4096, 4, 1024])
    cst_d = DI("cst", [128, 1152])
    rope_d = DI("rope", [128, NLT * 64])
    out_d = nc.dram_tensor("out", [OUT_ROWS, DM], F32, kind="ExternalOutput").ap()
    h2s_d = nc.dram_tensor("h2s", [OUT_ROWS, DM], BF16, kind="Internal").ap()
    slots_d = nc.dram_tensor("slots", [CAP, 2], I32, kind="Internal").ap()
    dbg_d = {}

    def DO(name, shape, dt=F32):
        dbg_d[name] = nc.dram_tensor(name, shape, dt, kind="ExternalOutput").ap()
        return dbg_d[name]

    with ExitStack() as top:
        S = Sched(nc, top)

        uid = [0]

        def SB(st, name, shape, dt):
            uid[0] += 1
            return st.enter_context(nc.sbuf_tensor("sb%d_%s" % (uid[0], name), shape, dt))

        def PS(st, name, shape, dt):
            uid[0] += 1
            return st.enter_context(nc.psum_tensor("ps%d_%s" % (uid[0], name), shape, dt))

        cst = SB(top, "cst", [128, 1152], F32)
        identf = cst[:, 0:128]
        maskU = cst[:, 128:256]
        maskL = cst[:, 256:384]
        segm = cst[:, 384:640]
        iota_p = cst[:, 640:641]
        blk128 = cst[:, 656:752]
        mU64 = cst[:, 896:1024]
        mL64 = cst[:, 1024:1152]
        ident = SB(top, "ident", [128, 128], BF16)
        ustr = SB(top, "ustr", [128, 128], BF16)
        ones_bf = SB(top, "ones_bf", [128, 128], BF16)
        epst = SB(top, "epst", [128, 1], F32)
        A1 = SB(top, "A1", [128, DM], F32)
        B1 = SB(top, "B1", [128, DM], F32)
        cA1 = SB(top, "cA1", [128, DM], F32)
        cB1 = SB(top, "cB1", [128, DM], F32)
        lbv = SB(top, "lbv", [128, 8], F32)
        omlb = SB(top, "omlb", [128, 8], F32)
        OF = SB(top, "OF", [128, NLT, 512], BF16)

        S.dma("sp", lambda e: e.dma_start(out=cst[:], in_=cst_d[:, :]), "cst", W=["cst"])
        S.op("dve", lambda e: e.tensor_copy(ident[:], identf), R=["cst"], W=["ident"])
        S.op("dve", lambda e: e.tensor_sub(ustr[:], maskU, identf), R=["cst"], W=["ustr"])
        S.op("dve", lambda e: e.memset(ones_bf[:], 1.0), W=["ones"])
        S.op("dve", lambda e: e.memset(epst[:], EPS), W=["eps"])

        def adaln_phase(cols, outs, lat_only_from):
            with ExitStack() as st:
                cv = SB(st, "cv", [128, 16], F32)
                cs = SB(st, "cs", [128, 16], F32)
                Lb = SB(st, "Lb", [128, 16, 128], F32)
                wa = [SB(st, "wa%d" % i, [128, 8, 512], F32) for i in range(2)]
                bb = SB(st, "bb", [128, 512], F32)
                pa = [PS(st, "pa%d" % i, [128, 512], F32) for i in range(2)]
                S.dma("sp", lambda e: e.dma_start(out=cv[:], in_=cvec_d[:, :]), "cv", W=["cv"])
                S.op("act", lambda e: e.activation(out=cs[:], in_=cv[:], func=AF.Silu), R=["cv"], W=["cs"])
                for j in range(16):
                    S.op("dve", lambda e, j=j: e.tensor_copy(Lb[:, j, :], cs[:, j:j + 1].to_broadcast([128, 128])),
                         R=["cs"], W=[("Lb", j)])
                for ci, n in enumerate(cols):
                    w = wa[ci % 2]
                    S.dma("sp", lambda e, w=w, n=n: e.dma_start(
                        out=w[:], in_=wada_d[:, n * 512:(n + 1) * 512].rearrange("(c p) n -> p c n", p=128)),
                        "wa%d" % (ci % 2), W=[("wa", ci % 2)])
                    S.dma("sp", lambda e, n=n: e.dma_start(
                        out=bb[:], in_=bada_d[0:1, n * 512:(n + 1) * 512].partition_broadcast(128)), "bb", W=["bb"])
                    for which in range(2):
                        dst = outs[n][which]
                        if dst is None:
                            continue
                        p = pa[which]
                        for k in range(8):
                            S.op("pe", lambda e, p=p, k=k, w=w, which=which: e.matmul(
                                p[:], lhsT=Lb[:, which * 8 + k, :], rhs=w[:, k, :], start=(k == 0), stop=(k == 7)),
                                R=[("Lb", which * 8 + k), ("wa", ci % 2)], W=[("pa", which)], signal=(k == 7))
                        S.op("dve", lambda e, p=p, dst=dst: e.tensor_add(dst, p[:], bb[:]),
                             R=["bb"], W=[("pa", which), ("mod", id(dst))])
            S.barrier()

        sh1 = B1
        with ExitStack() as st0:
            sc1 = SB(st0, "sc1", [128, DM], F32)
            csc1 = SB(st0, "csc1", [128, DM], F32)
            g1 = SB(st0, "g1", [128, DM], F32)
            outs = {0: (B1[:, 0:512], cB1[:, 0:512]), 1: (B1[:, 512:1024], cB1[:, 512:1024]),
                    2: (sc1[:, 0:512], csc1[:, 0:512]), 3: (sc1[:, 512:1024], csc1[:, 512:1024])}
            adaln_phase([0, 1, 2, 3], outs, None)
            S.dma("sp", lambda e: e.dma_start(out=g1[:], in_=n1g_d[0:1, :].partition_broadcast(128)), "g1", W=["g1"])
            S.op("dve", lambda e: e.scalar_tensor_tensor(out=A1[:], in0=sc1[:], scalar=1.0, in1=g1[:], op0=ALU.add, op1=ALU.mult),
                 R=["g1"], W=["A1"])
            S.op("dve", lambda e: e.scalar_tensor_tensor(out=cA1[:], in0=csc1[:], scalar=1.0, in1=g1[:], op0=ALU.add, op1=ALU.mult),
                 R=["g1"], W=["cA1"])
            lraw = SB(st0, "lraw", [128, 16], F32)
            S.dma("sp", lambda e: e.dma_start(out=lraw[:], in_=lbt_d[:, :]), "lraw", W=["lraw"])
            lr = lraw[:].rearrange("p (d l h) -> p d l h", d=2, l=2)
            ld = SB(st0, "ld", [128, 2, 4], F32)
            S.op("dve", lambda e: e.tensor_sub(ld[:], lr[:, :, 0, :], lr[:, :, 1, :]), R=["lraw"], W=["ld"])
            S.op("act", lambda e: e.activation(out=lbv[:].rearrange("p (d h) -> p d h", d=2), in_=ld[:], func=AF.Sigmoid),
                 R=["ld"], W=["lbv"])
            S.op("dve", lambda e: e.tensor_scalar(out=omlb[:], in0=lbv[:], scalar1=-1.0, scalar2=1.0, op0=ALU.mult, op1=ALU.add),
                 R=["lbv"], W=["omlb"])
            S.barrier()


        QO = SB(top, "QO", [128, 8, 4, 512], BF16)
        kvst = ExitStack()
        KTd = SB(kvst, "KTd", [128, 2, TT], BF16)
        Vaug = SB(kvst, "Vaug", [128, NT, 2, 66], BF16)
        S.op("dve", lambda e: e.memset(Vaug[:].rearrange("p a b c -> p (a b c)"), 1.0), W=["vones"])

        def pass_fb(dirn):
            with ExitStack() as st:
                Wq = SB(st, "Wq", [128, 8, 512], BF16)
                Wf = SB(st, "Wf", [128, 8, 512], BF16)
                Wi = SB(st, "Wi", [128, 8, 512], BF16)
                Wx = SB(st, "Wx", [128, 8, 768], BF16)
                fcol = 1280 if dirn == 0 else 1792
                for (w, c0, n, nm) in ((Wq, 768, 512, "Wq"), (Wf, fcol, 512, "Wf"), (Wi, 2304, 512, "Wi"),
                                      (Wx, 0 if dirn == 0 else 2816, 768 if dirn == 0 else 512, "Wx")):
                    S.dma("pool", lambda e, w=w, c0=c0, n=n: e.dma_start(
                        out=w[:, :, 0:n], in_=win_d[:, c0:c0 + n].rearrange("(c p) n -> p c n", p=128)), nm, W=[nm])
                if dirn == 0:
                    ropet = SB(st, "ropet", [128, NLT, 64], F32)
                    qkgain = SB(st, "qkgain", [128, 640], F32)
                    S.dma("sp", lambda e: e.dma_start(out=ropet[:].rearrange("p t c -> p (t c)"), in_=rope_d[:, :]), "ropet", W=["ropet"])
                    S.dma("sp", lambda e: e.dma_start(out=qkgain[:], in_=qkg_d[0:1, :].partition_broadcast(128)), "qkgain", W=["qkgain"])
                xt = [SB(st, "xt%d" % i, [128, DM], F32) for i in range(2)]
                tmp = SB(st, "tmp", [128, DM], F32)
                tmp_alias = tmp
                junk = tmp
                hb = SB(st, "hb", [128, DM], BF16)
                hTc = [SB(st, "hTc%d" % i, [128, 8, 2, 128], BF16) for i in range(2)]
                ss = SB(st, "ss", [128, 2], F32)
                rs = SB(st, "rs", [128, 2], F32)
                itm = [SB(st, "itm%d" % i, [128, 2, 512], BF16) for i in range(2)]
                if dirn == 1:
                    sg = [SB(st, "sg%d" % i, [128, 2, 512], BF16) for i in range(2)]
                    osum = SB(st, "osum", [128, 2, 512], F32)
                    hc = SB(st, "hc", [128, 256], F32)
                else:
                    sqb = tmp_alias
                    qn = SB(st, "qn", [128, 640], F32)
                    ss10 = SB(st, "ss10", [128, 10], F32)
                    rs10 = SB(st, "rs10", [128, 10], F32)
                    rt = [SB(st, "rt%d" % i, [128, 10, 32], F32) for i in range(2)] * 2
                    qkr = SB(st, "qkr", [128, 640], BF16)
                    kd2 = SB(st, "kd2", [128, 2, 128], BF16)
                hq = SB(st, "hq", [128, 256], F32)
                hf = SB(st, "hf", [128, 256], F32)
                hg = SB(st, "hg", [128, 256], F32)
                hk = SB(st, "hk", [128, 256], F32)
                hbb = SB(st, "hbb", [128, 256], F32)
                he = SB(st, "he", [128, 256], F32)
                ke = [SB(st, "ke%d" % i, [128, 256], BF16) for i in range(2)]
                Eend = [SB(st, "Eend%d" % i, [128, 4], F32) for i in range(2)]
                kdtm4 = SB(st, "kdtm4", [128, 4, 128], BF16)
                AT2 = SB(st, "AT2", [128, 2, 128], BF16)
                SB5 = SB(st, "SB5", [128, 5, 128], BF16)
                QEA = [SB(st, "QEA%d" % i, [128, 2, 128], BF16) for i in range(2)]
                QEB = [SB(st, "QEB%d" % i, [128, 2, 128], BF16) for i in range(2)]
                KDA = [SB(st, "KDA%d" % i, [128, 2, 128], BF16) for i in range(2)]
                KDB = [SB(st, "KDB%d" % i, [128, 2, 128], BF16) for i in range(2)]
                S32 = SB(st, "S32", [128, 4, 128], F32)
                ss4 = SB(st, "ss4", [128, 4], F32)
                rs4 = SB(st, "rs4", [128, 4], F32)
                ptr = PS(st, "ptr", [128, DM], BF16)
                psq = PS(st, "psq", [128, 512], F32)
                pskv = PS(st, "pskv", [128, 512], F32)
                psi = PS(st, "psi", [128, 512], F32)
                pfa = PS(st, "pfa", [128, 512], F32)
                pfb = PS(st, "pfb", [128, 512], F32)
                psh = PS(st, "psh", [128, 512], F32)
                pTb = PS(st, "pTb", [128, DM], BF16)
                S.op("dve", lambda e: e.memset(S32[:].rearrange("p a b -> p (a b)"), 0.0), W=["S32"])
                mask = mU64 if dirn == 0 else mL64
                endc = 63 if dirn == 0 else 0
                for nm_, t_ in (("QEA", QEA), ("QEB", QEB), ("KDA", KDA), ("KDB", KDB)):
                    for pb_ in range(2):
                        S.op("dve", lambda e, t_=t_, pb_=pb_: e.memset(t_[pb_][:].rearrange("p a b -> p (a b)"), 0.0), W=[(nm_, pb_)])
                chunks = list(range(17)) if dirn == 0 else [0] + list(range(16, 0, -1))
                if (dirn == 1 and stage == 2) or stage < 2:
                    chunks = chunks[:LIM[0]]
                def prologue(ci, cb):
                    for tl in range(2):
                        gt = ci * 2 + tl
                        isctx = gt < 2
                        lt = gt - 2
                        xs = xt[gt % 2]
                        src = ctx_d[gt * 128:(gt + 1) * 128, :] if isctx else x_d[lt * 128:(lt + 1) * 128, :]
                        S.dma("sp", lambda e, xs=xs, src=src: e.dma_start(out=xs[:], in_=src), "xt%d" % (gt % 2), W=[("xt", gt % 2)])
                        S.op("act", lambda e, xs=xs, tl=tl: e.activation(out=junk[:], in_=xs[:], func=AF.Square, accum_out=ss[:, tl:tl + 1]),
                             R=[("xt", gt % 2)], W=["tmp", ("ss", tl)])
                        S.op("act", lambda e, tl=tl: e.activation(out=rs[:, tl:tl + 1], in_=ss[:, tl:tl + 1], func=AF.Sqrt, scale=1.0 / DM, bias=epst[:]),
                             R=[("ss", tl), "eps"], W=[("rs", tl)])
                        S.op("dve", lambda e, tl=tl: e.reciprocal(rs[:, tl:tl + 1], rs[:, tl:tl + 1]), R=[("rs", tl)], W=[("rs", tl)])
                        Am, Bm = (cA1, cB1) if isctx else (A1, B1)
                        S.op("dve", lambda e, xs=xs, tl=tl, Am=Am: e.scalar_tensor_tensor(
                            out=tmp[:], in0=xs[:], scalar=rs[:, tl:tl + 1], in1=Am[:], op0=ALU.mult, op1=ALU.mult),
                            R=[("xt", gt % 2), ("rs", tl)], W=["tmp"])
                        S.op("pool", lambda e, Bm=Bm: e.tensor_add(hb[:], tmp[:], Bm[:]), R=["tmp"], W=["hb"])
                        for k in range(8):
                            S.op("pe", lambda e, k=k: e.transpose(ptr[:, k * 128:(k + 1) * 128], hb[:, k * 128:(k + 1) * 128], ident[:]),
                                 R=["hb", "ident"], W=["ptr"], signal=(k == 7))
                        S.op("act", lambda e, tl=tl: e.activation(out=hTc[cb][:, :, tl, :], in_=ptr[:].rearrange("p (k t) -> p k t", k=8), func=AF.Copy),
                             W=["ptr", ("hTc", cb, tl)])
                        for k in range(8):
                            lh = hTc[cb][:, k, tl, :]
                            if dirn == 0:
                                if not isctx:
                                    S.op("pe", lambda e, k=k, lh=lh: e.matmul(psq[:], lhsT=lh, rhs=Wx[:, k, 0:512], start=(k == 0), stop=(k == 7)),
                                         R=[("hTc", cb, tl), "Wx"], W=["psq"], signal=False)
                                S.op("pe", lambda e, k=k, lh=lh: e.matmul(pskv[:, 0:256], lhsT=lh, rhs=Wx[:, k, 512:768], start=(k == 0), stop=(k == 7)),
                                     R=[("hTc", cb, tl), "Wx"], W=["pskv"], signal=False)
                            elif not isctx:
                                S.op("pe", lambda e, k=k, lh=lh: e.matmul(pskv[:], lhsT=lh, rhs=Wx[:, k, 0:512], start=(k == 0), stop=(k == 7)),
                                     R=[("hTc", cb, tl), "Wx"], W=["pskv"], signal=False)
                            S.op("pe", lambda e, k=k, lh=lh: e.matmul(psi[:], lhsT=lh, rhs=Wi[:, k, :], start=(k == 0), stop=(k == 7)),
                                 R=[("hTc", cb, tl), "Wi"], W=["psi"], signal=(k == 7))
                        S.op("act", lambda e, tl=tl: e.activation(out=itm[cb][:, tl, :], in_=psi[:], func=AF.Copy), W=["psi", ("itm", cb, tl)])
                        if dirn == 1 and not isctx:
                            S.op("act", lambda e, tl=tl: e.activation(out=sg[cb][:, tl, :], in_=pskv[:], func=AF.Silu), W=["pskv", ("sg", cb, tl)])
                        if dirn == 0 and not (LIM[1] & 1):
                            S.op("act", lambda e, gt=gt: e.activation(out=Vaug[:, gt, :, 0:64], in_=pskv[:, 128:256].rearrange("p (a d) -> p a d", a=2), func=AF.Copy),
                                 W=["pskv", ("V", gt)])
                            nh = 2 if isctx else 10
                            o0 = 512 if isctx else 0
                            if not isctx:
                                S.op("act", lambda e: e.activation(out=sqb[:, 0:512], in_=psq[:], func=AF.Square), W=["psq", "sqbq", "tmp"])
                            S.op("act", lambda e: e.activation(out=sqb[:, 512:640], in_=pskv[:, 0:128], func=AF.Square), W=["pskv", "sqbk", "tmp"])
                            S.op("dve", lambda e, o0=o0: e.tensor_reduce(out=ss10[:, o0 // 64:10], in_=sqb[:, o0:640].rearrange("p (h d) -> p h d", d=64), axis=AX.X, op=ALU.add),
                                 R=["sqbq", "sqbk", "tmp"], W=["ss10"])
                            S.op("act", lambda e, o0=o0: e.activation(out=rs10[:, o0 // 64:10], in_=ss10[:, o0 // 64:10], func=AF.Sqrt, scale=1.0 / 64, bias=epst[:]),
                                 R=["ss10", "eps"], W=["rs10"])
                            S.op("dve", lambda e, o0=o0: e.reciprocal(rs10[:, o0 // 64:10], rs10[:, o0 // 64:10]), R=["rs10"], W=["rs10"])
                            if not isctx:
                                S.op("dve", lambda e: e.tensor_tensor(out=qn[:, 0:512].rearrange("p (h d) -> p h d", d=64), in0=psq[:].rearrange("p (h d) -> p h d", d=64),
                                                                      in1=rs10[:, 0:8].unsqueeze(2).to_broadcast([128, 8, 64]), op=ALU.mult),
                                     R=["rs10"], W=["psq", "qnq"])
                            S.op("dve", lambda e: e.tensor_tensor(out=qn[:, 512:640].rearrange("p (h d) -> p h d", d=64), in0=pskv[:, 0:128].rearrange("p (h d) -> p h d", d=64),
                                                                  in1=rs10[:, 8:10].unsqueeze(2).to_broadcast([128, 2, 64]), op=ALU.mult),
                                 R=["rs10"], W=["pskv", "qnk"])
                            S.op("pool", lambda e, o0=o0: e.tensor_mul(qn[:, o0:640], qn[:, o0:640], qkgain[:, o0:640]), R=["qnq", "qnk", "qkgain"], W=["qn"])
                            if isctx:
                                S.op("dve", lambda e: e.tensor_copy(kd2[:, :, 0:64], qn[:, 512:640].rearrange("p (h d) -> p h d", d=64)), R=["qn"], W=["kd2a"])
                                S.op("dve", lambda e: e.tensor_copy(kd2[:, :, 64:128], qn[:, 512:640].rearrange("p (h d) -> p h d", d=64)), R=["qn"], W=["kd2b"])
                            else:
                                qv = qn[:].rearrange("p (h c two) -> p h c two", h=10, two=2)
                                x1, x2 = qv[:, :, :, 0], qv[:, :, :, 1]
                                cosb = ropet[:, lt, 0:32].unsqueeze(1).to_broadcast([128, 10, 32])
                                sinb = ropet[:, lt, 32:64].unsqueeze(1).to_broadcast([128, 10, 32])
                                S.op("dve", lambda e: e.tensor_tensor(out=rt[0][:], in0=x1, in1=cosb, op=ALU.mult), R=["qn", "ropet"], W=["rt0"])
                                S.op("dve", lambda e: e.tensor_tensor(out=rt[1][:], in0=x2, in1=sinb, op=ALU.mult), R=["qn", "ropet"], W=["rt1"])
                                qo = qkr[:].rearrange("p (h c two) -> p h c two", h=10, two=2)
                                S.op("dve", lambda e: e.tensor_sub(qo[:, :, :, 0], rt[0][:], rt[1][:]), R=["rt0", "rt1"], W=["qkr0"])
                                S.op("dve", lambda e: e.tensor_tensor(out=rt[2][:], in0=x1, in1=sinb, op=ALU.mult), R=["qn", "ropet"], W=["rt0"])
                                S.op("dve", lambda e: e.tensor_tensor(out=rt[3][:], in0=x2, in1=cosb, op=ALU.mult), R=["qn", "ropet"], W=["rt1"])
                                S.op("dve", lambda e: e.tensor_add(qo[:, :, :, 1], rt[2][:], rt[3][:]), R=["rt0", "rt1"], W=["qkr1"])
                                S.op("dve", lambda e: e.tensor_copy(kd2[:, :, 0:64], qkr[:, 512:640].rearrange("p (h d) -> p h d", d=64)), R=["qkr0", "qkr1"], W=["kd2a"])
                                S.op("dve", lambda e: e.tensor_copy(kd2[:, :, 64:128], qkr[:, 512:640].rearrange("p (h d) -> p h d", d=64)), R=["qkr0", "qkr1"], W=["kd2b"])
                                for j in range(4):
                                    S.op("pe", lambda e, j=j: e.transpose(pTb[:, j * 128:(j + 1) * 128], qkr[:, j * 128:(j + 1) * 128], ident[:]),
                                         R=["qkr0", "qkr1", "ident"], W=["pTb"], signal=(j == 3))
                                S.op("act", lambda e, lt=lt: e.activation(out=QO[:, lt // 4, :, (lt % 4) * 128:(lt % 4 + 1) * 128], in_=pTb[:, 0:512].rearrange("p (j t) -> p j t", j=4), func=AF.Copy),
                                     W=["pTb", ("QO", lt // 4)])
                            for kv in range(2):
                                S.op("pe", lambda e, kv=kv: e.transpose(pTb[:, 512 + kv * 128:512 + (kv + 1) * 128], kd2[:, kv, :], ident[:]),
                                     R=["kd2a", "kd2b", "ident"], W=["pTb"], signal=(kv == 1))
                            S.op("act", lambda e, gt=gt: e.activation(out=KTd[:, :, gt * 128:(gt + 1) * 128], in_=pTb[:, 512:768].rearrange("p (j t) -> p j t", j=2), func=AF.Copy),
                                 W=["pTb", ("KT", gt)])
                def hgrn(ci, cb, pend):
                    pts = [44]

                    def fl():
                        if pend:
                            S.flush(pend, -(-len(pend) // max(pts[0], 1)))
                        pts[0] -= 1
                    def prep(hd, pb):
                        S.atomic_begin()
                        for k in range(8):
                            rh = hTc[cb][:, k, :, :].rearrange("p a t -> p (a t)")
                            S.op("pe", lambda e, k=k, rh=rh, hd=hd: e.matmul(pfa[:, 0:256], lhsT=Wq[:, k, hd * 128:(hd + 1) * 128], rhs=rh, start=(k == 0), stop=(k == 7)),
                                 R=[("hTc", cb, 0), ("hTc", cb, 1), "Wq"], W=["pfa"], signal=False)
                            S.op("pe", lambda e, k=k, rh=rh, hd=hd: e.matmul(pfb[:, 0:256], lhsT=Wf[:, k, hd * 128:(hd + 1) * 128], rhs=rh, start=(k == 0), stop=(k == 7)),
                                 R=[("hTc", cb, 0), ("hTc", cb, 1), "Wf"], W=["pfb"], signal=(k == 7))
                        S.atomic_end()
                        li = dirn * 4 + hd
                        S.op("act", lambda e: e.activation(out=hq[:], in_=pfa[:, 0:256], func=AF.Silu), W=["pfa", "hq"])
                        S.op("act", lambda e: e.activation(out=hf[:], in_=pfb[:, 0:256], func=AF.Sigmoid), W=["pfb", "hf"])
                        S.op("dve", lambda e, li=li: e.tensor_scalar(out=hf[:], in0=hf[:], scalar1=omlb[:, li:li + 1], scalar2=lbv[:, li:li + 1], op0=ALU.mult, op1=ALU.add),
                             R=["hf", "lbv", "omlb"], W=["hf"])
                        S.op("act", lambda e: e.activation(out=hg[:], in_=hf[:], func=AF.Ln), R=["hf"], W=["hg"])
                        if S.defer is None:
                            fl()
                        S.op("pool", lambda e: e.tensor_scalar(out=hk[:], in0=hf[:], scalar1=-1.0, scalar2=1.0, op0=ALU.mult, op1=ALU.add), R=["hf"], W=["hk"])
                        S.op("dve", lambda e: e.tensor_tensor_scan(out=hbb[:], data0=segm, data1=hg[:], initial=0.0, op0=ALU.mult, op1=ALU.add),
                             R=["hg", "cst"], W=["hbb"])
                        b4 = hbb[:].rearrange("p (c t) -> p c t", t=64)
                        if dirn == 0:
                            cum, cumk = hbb, "hbb"
                        else:
                            S.op("pool", lambda e: e.tensor_sub(hc[:], hg[:], hbb[:]), R=["hg", "hbb"], W=["hc"])
                            S.op("dve", lambda e: e.tensor_tensor(out=hc[:].rearrange("p (c t) -> p c t", t=64), in0=hc[:].rearrange("p (c t) -> p c t", t=64),
                                                                  in1=b4[:, :, 63:64].to_broadcast([128, 4, 64]), op=ALU.add), R=["hc", "hbb"], W=["hc"])
                            cum, cumk = hc, "hc"
                        c4 = cum[:].rearrange("p (c t) -> p c t", t=64)
                        if S.defer is None:
                            fl()
                        S.op("act", lambda e: e.activation(out=Eend[pb][:].unsqueeze(2), in_=c4[:, :, endc:endc + 1], func=AF.Exp), R=[cumk], W=[("Eend", pb)])
                        S.op("act", lambda e: e.activation(out=hg[:], in_=cum[:], func=AF.Exp), R=[cumk], W=["hg"])
                        hq4 = hq[:].rearrange("p (a c t) -> p a c t", a=2, c=2)
                        hg4 = hg[:].rearrange("p (a c t) -> p a c t", a=2, c=2)
                        S.op("dve", lambda e: e.scalar_tensor_tensor(out=QEA[pb][:, :, 0:64], in0=hq4[:, :, 0, :], scalar=float(128 ** -0.5), in1=hg4[:, :, 0, :], op0=ALU.mult, op1=ALU.mult),
                             R=["hq", "hg"], W=[("QEA", pb)])
                        S.op("dve", lambda e: e.scalar_tensor_tensor(out=QEB[pb][:, :, 64:128], in0=hq4[:, :, 1, :], scalar=float(128 ** -0.5), in1=hg4[:, :, 1, :], op0=ALU.mult, op1=ALU.mult),
                             R=["hq", "hg"], W=[("QEB", pb)])
                        if S.defer is None:
                            fl()
                        S.op("act", lambda e: e.activation(out=hf[:], in_=cum[:], func=AF.Exp, scale=-1.0), R=[cumk], W=["hf"])
                        S.op("pool", lambda e: e.tensor_mul(ke[pb][:], hk[:], hf[:]), R=["hk", "hf"], W=[("ke", pb)])
                        if S.defer is None:
                            fl()
                        S.op("dve", lambda e: e.tensor_tensor(out=he[:].rearrange("p (c t) -> p c t", t=64), in0=c4, in1=c4[:, :, endc:endc + 1].to_broadcast([128, 4, 64]), op=ALU.subtract),
                             R=[cumk], W=["he"])
                        S.op("act", lambda e: e.activation(out=hq[:], in_=he[:], func=AF.Exp, scale=-1.0), R=["he", ("QEA", pb), ("QEB", pb)], W=["hq"])
                        hk4 = hk[:].rearrange("p (a c t) -> p a c t", a=2, c=2)
                        S.op("dve", lambda e: e.tensor_tensor(out=KDA[pb][:, :, 0:64], in0=hk4[:, :, 0, :], in1=hq4[:, :, 0, :], op=ALU.mult), R=["hk", "hq"], W=[("KDA", pb)])
                        S.op("dve", lambda e: e.tensor_tensor(out=KDB[pb][:, :, 64:128], in0=hk4[:, :, 1, :], in1=hq4[:, :, 1, :], op=ALU.mult), R=["hk", "hq"], W=[("KDB", pb)])
                        if S.defer is None:
                            fl()
                    def phases(hd, pb, pend2):
                        pts2 = [6]

                        def fl2():
                            if pend2:
                                S.flush(pend2, -(-len(pend2) // max(pts2[0], 1)))
                            pts2[0] -= 1
                        hsl = slice(hd * 128, (hd + 1) * 128)
                        tls = (0, 1) if dirn == 0 else (1, 0)
                        cs = (0, 1) if dirn == 0 else (1, 0)
                        lat = ci > 0
                        lt0 = ci * 2 - 2
                        for q in range(4):
                            KDc, kk_ = (KDA[pb], ("KDA", pb)) if q % 2 == 0 else (KDB[pb], ("KDB", pb))
                            S.op("pe", lambda e: e.transpose(pTb[:, q * 128:(q + 1) * 128], KDc[:, q // 2, :], ident[:]), R=[kk_, "ident"], W=["pTb"], signal=(q == 3))
                        S.op("act", lambda e: e.activation(out=kdtm4[:].rearrange("p a b -> p (a b)"), in_=pTb[:, 0:512], func=AF.Copy), W=["pTb", "kdtm4"])
                        Ureg = [(pfa, 256, "pfa"), (pfa, 384, "pfa"), (pfb, 256, "pfb"), (pfb, 384, "pfb")]
                        for q in range(4):
                            ub, uo, uk = Ureg[q]
                            S.op("pe", lambda e: e.matmul(ub[:, uo:uo + 128], lhsT=kdtm4[:, q, :], rhs=itm[cb][:, q // 2, hsl], start=True, stop=True),
                                 R=["kdtm4", ("itm", cb, q // 2)], W=[uk], signal=(q % 2 == 1))
                        if lat:
                            for tl in range(2):
                                tsl = slice(tl * 128, (tl + 1) * 128)
                                S.op("pe", lambda e: e.matmul(psh[:, tsl], lhsT=ke[pb][:, tsl], rhs=QEA[pb][:, tl, :], start=True, stop=False), R=[("ke", pb), ("QEA", pb)], W=["psh"], signal=False)
                                S.op("pe", lambda e: e.matmul(psh[:, tsl], lhsT=ke[pb][:, tsl], rhs=QEB[pb][:, tl, :], start=False, stop=True), R=[("ke", pb), ("QEB", pb)], W=["psh"], signal=(tl == 1))
                            S.op("dve", lambda e: e.tensor_tensor(out=AT2[:], in0=psh[:, 0:256].rearrange("p (a t) -> p a t", a=2),
                                                                  in1=mask.unsqueeze(1).to_broadcast([128, 2, 128]), op=ALU.mult), R=["cst"], W=["psh", "AT2"])
                        fl()
                        fl2()
                        order = [(tl, c) for tl in tls for c in cs]
                        S.op("pool", lambda e: e.tensor_copy(SB5[:, 0, :], S32[:, hd, :]), R=["S32"], W=[("SB5", 0)])
                        for i, (tl, c) in enumerate(order):
                            q = tl * 2 + c
                            ub, uo, uk = Ureg[q]
                            S.op("dve", lambda e: e.scalar_tensor_tensor(out=SB5[:, i + 1, :], in0=S32[:, hd, :], scalar=Eend[pb][:, q:q + 1], in1=ub[:, uo:uo + 128], op0=ALU.mult, op1=ALU.add),
                                 R=["S32", ("Eend", pb)], W=[uk, ("SB5", i + 1)])
                            S.op("dve", lambda e: e.scalar_tensor_tensor(out=S32[:, hd, :], in0=S32[:, hd, :], scalar=Eend[pb][:, q:q + 1], in1=ub[:, uo:uo + 128], op0=ALU.mult, op1=ALU.add),
                                 R=[("Eend", pb)], W=[uk, "S32"])
                            fl()
                            fl2()
                        fl2()
                        if lat:
                            for tl in tls:
                                i0, i1 = order.index((tl, cs[0])), order.index((tl, cs[1]))
                                QE0, q0k = (QEA[pb], ("QEA", pb)) if cs[0] == 0 else (QEB[pb], ("QEB", pb))
                                QE1, q1k = (QEA[pb], ("QEA", pb)) if cs[1] == 0 else (QEB[pb], ("QEB", pb))
                                osl = slice(256 + tl * 128, 256 + (tl + 1) * 128)
                                S.op("pe", lambda e: e.matmul(psh[:, osl], lhsT=QE0[:, tl, :], rhs=SB5[:, i0, :], start=True, stop=False), R=[q0k, ("SB5", i0)], W=["psh"], signal=False)
                                S.op("pe", lambda e: e.matmul(psh[:, osl], lhsT=QE1[:, tl, :], rhs=SB5[:, i1, :], start=False, stop=False), R=[q1k, ("SB5", i1)], W=["psh"], signal=False)
                                S.op("pe", lambda e: e.matmul(psh[:, osl], lhsT=AT2[:, tl, :], rhs=itm[cb][:, tl, hsl], start=False, stop=True), R=["AT2", ("itm", cb, tl)], W=["psh"])
                            po3 = psh[:, 256:512].rearrange("p (a t) -> p a t", a=2)
                            if dirn == 0:
                                S.op("act", lambda e: e.activation(out=OF[:, lt0:lt0 + 2, hsl], in_=po3, func=AF.Copy), W=["psh", ("OF", lt0, hd), ("OF", lt0 + 1, hd)])
                            else:
                                S.op("dve", lambda e: e.tensor_tensor(out=osum[:, :, hsl], in0=po3, in1=OF[:, lt0:lt0 + 2, hsl], op=ALU.add),
                                     R=[("OF", lt0, hd), ("OF", lt0 + 1, hd)], W=["psh", ("osum", 0, hd), ("osum", 1, hd)])
                        fl()
                        fl2()
                        S.flush(pend2)
                    heads = list(range(4)) if not (LIM[1] & 2) else []
                    if heads:
                        prep(0, 0)
                    for hd in heads:
                        pend2 = []
                        if hd + 1 < 4:
                            S.defer = pend2
                            prep(hd + 1, (hd + 1) % 2)
                            S.defer = None
                        phases(hd, hd % 2, pend2)
                    if dirn == 1 and ci > 0:
                        for tl in range(2):
                            lt = ci * 2 + tl - 2
                            S.op("act", lambda e, tl=tl: e.activation(out=junk[:, 0:512], in_=osum[:, tl, :], func=AF.Square), R=[("osum", tl, h_) for h_ in range(4)], W=["tmp"])
                            S.op("dve", lambda e: e.tensor_reduce(out=ss4[:], in_=junk[:, 0:512].rearrange("p (h d) -> p h d", d=128), axis=AX.X, op=ALU.add), R=["tmp"], W=["ss4"])
                            S.op("act", lambda e: e.activation(out=rs4[:], in_=ss4[:], func=AF.Sqrt, scale=1.0 / 128, bias=epst[:]), R=["ss4", "eps"], W=["rs4"])
                            S.op("dve", lambda e: e.reciprocal(rs4[:], rs4[:]), R=["rs4"], W=["rs4"])
                            S.op("dve", lambda e, tl=tl: e.tensor_tensor(out=junk[:, 512:1024].rearrange("p (h d) -> p h d", d=128), in0=osum[:, tl, :].rearrange("p (h d) -> p h d", d=128),
                                                                         in1=rs4[:].unsqueeze(2).to_broadcast([128, 4, 128]), op=ALU.mult), R=["rs4"] + [("osum", tl, h_) for h_ in range(4)], W=["tmp"])
                            S.op("pool", lambda e, tl=tl, lt=lt: e.tensor_mul(OF[:, lt, :], junk[:, 512:1024], sg[cb][:, tl, :]), R=["tmp", ("sg", cb, tl)], W=[("OF", lt, h_) for h_ in range(4)])
                    S.flush(pend)
                prologue(chunks[0], 0)
                for idx, ci in enumerate(chunks):
                    cb = idx % 2
                    pend = []
                    if idx + 1 < len(chunks):
                        S.defer = pend
                        prologue(chunks[idx + 1], 1 - cb)
                        S.defer = None
                    hgrn(ci, cb, pend)
                if dbg and stage == 2 and dirn == 1:
                    S.barrier()
                    S.dma("sp", lambda e: e.dma_start(out=DO("d_osum", [128, 1024], F32)[:, :], in_=osum[:].rearrange("p a b -> p (a b)")), "dd_osum")
                    S.dma("sp", lambda e: e.dma_start(out=DO("d_sg", [128, 1024], BF16)[:, :], in_=sg[0][:].rearrange("p a b -> p (a b)")), "dd_sg")
                    for nm, t, n, dt in (("hbb", hbb, 256, F32), ("hc", hc, 256, F32), ("hk", hk, 256, F32), ("Eend", Eend[1], 4, F32)):
                        S.dma("sp", lambda e, nm=nm, t=t, n=n, dt=dt: e.dma_start(out=DO("d_" + nm, [128, n], dt)[:, :], in_=t[:]), "dd_" + nm)
                    S.dma("sp", lambda e: e.dma_start(out=DO("d_S32", [128, 512], F32)[:, :], in_=S32[:].rearrange("p a b -> p (a b)")), "dd_S32")
                    S.dma("sp", lambda e: e.dma_start(out=DO("d_QEA", [128, 256], BF16)[:, :], in_=QEA[0][:].rearrange("p a b -> p (a b)")), "dd_QEA")
                    S.dma("sp", lambda e: e.dma_start(out=DO("d_KDB", [128, 256], BF16)[:, :], in_=KDB[0][:].rearrange("p a b -> p (a b)")), "dd_KDB")
            S.barrier()

        if stage >= 1:
            pass_fb(0)
        if dbg and stage == 1:
            S.dma("sp", lambda e: e.dma_start(out=DO("d_QT", [128, 4 * SEQ], BF16)[:, :], in_=QO[:].rearrange("p a b t -> p (a b t)")), "o0", R=[])
            S.dma("sp", lambda e: e.dma_start(out=DO("d_KT", [128, 2 * TT], BF16)[:, :], in_=KTd[:].rearrange("p a t -> p (a t)")), "o1", R=[])
            S.dma("sp", lambda e: e.dma_start(out=DO("d_V", [128, NT * 132], BF16)[:, :], in_=Vaug[:].rearrange("p a b c -> p (a b c)")), "o2", R=[])
            S.dma("sp", lambda e: e.dma_start(out=DO("d_OF", [128, NLT * 512], BF16)[:, :], in_=OF[:].rearrange("p a t -> p (a t)")), "o3", R=[])
            S.barrier()
        if stage >= 2:
            pass_fb(1)
            if dbg and stage == 2:
                S.dma("sp", lambda e: e.dma_start(out=DO("d_OF", [128, NLT * 512], BF16)[:, :], in_=OF[:].rearrange("p a t -> p (a t)")), "o3", R=[])
                S.barrier()


        def attention():
            with ExitStack() as st:
                PT = [[SB(st, "PT%d_%d" % (a_, i), [128, 512], BF16) for i in range(4)] for a_ in range(2)]
                OT = [SB(st, "OT%d" % i, [65, 512], F32) for i in range(2)]
                OA = SB(st, "OA", [128, 4, 8, 65], F32)
                rl = SB(st, "rl", [128, 8], F32)
                ss8 = SB(st, "ss8", [128, 8], F32)
                sc8 = SB(st, "sc8", [128, 8], F32)
                ajunk = SB(st, "ajunk", [128, 8, 64], F32)
                pS = [[PS(st, "pS%d_%d" % (a_, i), [128, 512], F32) for i in range(2)] for a_ in range(2)]
                pO = [PS(st, "pO%d" % i, [128, 512], F32) for i in range(2)]
                pOT = [PS(st, "pOT%d" % i, [128, 512], F32) for i in range(2)]
                for qb in range(8 if (LIM[0] > 8 or stage != 3) else LIM[0]):
                    for pair in range(4):
                        kv = pair // 2

                        def qk(g):
                            for a_ in range(2):
                                hp = a_ * 64
                                S.op("pe", lambda e: e.matmul(pS[a_][g % 2][:], lhsT=KTd[hp:hp + 64, kv, g * 128:(g + 1) * 128],
                                                              rhs=QO[hp:hp + 64, qb, pair, :], start=True, stop=True),
                                     R=[("QO", qb)], W=[("pS", a_, g % 2)], signal=(a_ == 1))
                        qk(0)
                        for g in range(NT):
                            if g + 1 < NT:
                                qk(g + 1)
                            for a_ in range(2):
                                S.op("act", lambda e: e.activation(out=PT[a_][g % 4][:], in_=pS[a_][g % 2][:], func=AF.Exp, scale=0.125),
                                     W=[("pS", a_, g % 2), ("PT", a_, g % 4)])
                            for a_ in range(2):
                                S.op("pe", lambda e: e.matmul(pO[a_][0:65, :], lhsT=Vaug[:, g, kv, 0:65], rhs=PT[a_][g % 4][:], start=(g == 0), stop=(g == NT - 1)),
                                     R=[("PT", a_, g % 4)], W=[("pO", a_)], signal=(g == NT - 1 or a_ == 1))
                        for a_ in range(2):
                            h = pair * 2 + a_
                            S.op("dve", lambda e: e.tensor_copy(OT[a_][:], pO[a_][0:65, :]), W=[("pO", a_), ("OT", a_)])
                            for j in range(4):
                                S.op("pe", lambda e: e.transpose(pOT[a_][:, j * 65:(j + 1) * 65], OT[a_][0:65, j * 128:(j + 1) * 128], identf[0:65, 0:65]),
                                     R=[("OT", a_), "cst"], W=[("pOT", a_)], signal=(j == 3))
                            S.op("dve", lambda e: e.tensor_copy(OA[:, :, h, :], pOT[a_][:, 0:260].rearrange("p (j c) -> p j c", j=4)), W=[("pOT", a_), ("OA", h)])
                    for j in range(4):
                        oaj = OA[:, j, :, 0:64]
                        S.op("dve", lambda e: e.reciprocal(rl[:], OA[:, j, :, 64]), R=[("OA", h_) for h_ in range(8)], W=["rl"])
                        S.op("dve", lambda e: e.tensor_tensor(out=ajunk[:], in0=oaj, in1=oaj, op=ALU.mult), R=[("OA", h_) for h_ in range(8)], W=["ajunk"])
                        S.op("dve", lambda e: e.tensor_reduce(out=ss8[:], in_=ajunk[:], axis=AX.X, op=ALU.add), R=["ajunk"], W=["ss8"])
                        S.op("dve", lambda e: e.tensor_mul(ss8[:], ss8[:], rl[:]), R=["ss8", "rl"], W=["ss8"])
                        S.op("dve", lambda e: e.tensor_mul(ss8[:], ss8[:], rl[:]), R=["ss8", "rl"], W=["ss8"])
                        S.op("act", lambda e: e.activation(out=sc8[:], in_=ss8[:], func=AF.Sqrt, scale=1.0 / 64, bias=epst[:]), R=["ss8", "eps"], W=["sc8"])
                        S.op("dve", lambda e: e.reciprocal(sc8[:], sc8[:]), R=["sc8"], W=["sc8"])
                        S.op("dve", lambda e: e.tensor_mul(sc8[:], sc8[:], rl[:]), R=["sc8", "rl"], W=["sc8"])
                        S.op("dve", lambda e: e.tensor_tensor(out=QO[:, qb, j, :].rearrange("p (h d) -> p h d", d=64), in0=oaj,
                                                              in1=sc8[:].unsqueeze(2).to_broadcast([128, 8, 64]), op=ALU.mult),
                             R=["sc8"] + [("OA", h_) for h_ in range(8)], W=[("QO", qb)])
            S.barrier()

        if stage >= 3:
            attention()
            if dbg and stage == 3:
                S.dma("sp", lambda e: e.dma_start(out=DO("d_OAN", [128, 4 * SEQ], BF16)[:, :], in_=QO[:].rearrange("p a b t -> p (a b t)")), "o0", R=[])
                S.barrier()


        kvst.close()
        G1, B2, A2, G2 = A1, B1, cA1, cB1
        M1 = SB(top, "M1", [128, NLT, 32], F32)
        M2 = SB(top, "M2", [128, NLT, 32], F32)
        Msum = SB(top, "Msum", [128, NLT, 32], BF16)
        W12 = SB(top, "W12", [128, NLT, 2], F32)
        SLOT = SB(top, "SLOT", [128, NBLK, 2], I32)
        IDXW = SB(top, "IDXW", [128, NBLK], I32)
        IDXS = SB(top, "IDXS", [128, 2, NBLK], I32)

        def phase_d():
            with ExitStack() as st0:
                sc2 = SB(st0, "sc2", [128, DM], F32)
                g2 = SB(st0, "g2", [128, DM], F32)
                outs = {4: (G1[:, 0:512], None), 5: (G1[:, 512:1024], None), 6: (B2[:, 0:512], None), 7: (B2[:, 512:1024], None),
                        8: (sc2[:, 0:512], None), 9: (sc2[:, 512:1024], None), 10: (G2[:, 0:512], None), 11: (G2[:, 512:1024], None)}
                adaln_phase(list(range(4, 12)), outs, None)
                S.dma("sp", lambda e: e.dma_start(out=g2[:], in_=n2g_d[0:1, :].partition_broadcast(128)), "g2", W=["g2"])
                S.op("dve", lambda e: e.scalar_tensor_tensor(out=A2[:], in0=sc2[:], scalar=1.0, in1=g2[:], op0=ALU.add, op1=ALU.mult), R=["g2"], W=["A2"])
                S.barrier()
            with ExitStack() as st:
                Wout = SB(st, "Wout", [128, 8, DM], BF16)
                stg = [SB(st, "stg%d" % i, [128, DM], F32) for i in range(2)]
                aog = SB(st, "aog", [128, 8], F32)
                Wr = SB(st, "Wr", [128, 8, 36], BF16)
                brb = SB(st, "brb", [128, 36], F32)
                catT = [SB(st, "catT%d" % i, [128, 8, 128], BF16) for i in range(2)]
                xt = [SB(st, "dxt%d" % i, [128, DM], F32) for i in range(2)]
                x1 = [SB(st, "x1%d" % i, [128, DM], F32) for i in range(2)]
                tmp = SB(st, "dtmp", [128, DM], F32)
                h2b = [SB(st, "h2b%d" % i, [128, DM], BF16) for i in range(2)]
                h2T = SB(st, "h2T", [128, 8, 128], BF16)
                zt = SB(st, "zt", [128, DM], BF16)
                ss = SB(st, "dss", [128, 1], F32)
                rs = SB(st, "drs", [128, 1], F32)
                LG = SB(st, "LG", [128, NLT, 36], F32)
                gmx = SB(st, "gmx", [128, NLT], F32)
                pgv = SB(st, "pgv", [128, NLT], F32)
                ohg = SB(st, "ohg", [128, NLT, 4], F32)
                eg = SB(st, "eg", [128, NLT, 4], F32)
                prod = SB(st, "prod", [128, NLT, 8, 4], F32)
                les = SB(st, "les", [128, NLT, 8], F32)
                les2 = SB(st, "les2", [128, NLT, 8], F32)
                oh1 = SB(st, "oh1", [128, NLT, 8], F32)
                oh2 = SB(st, "oh2", [128, NLT, 8], F32)
                m1 = SB(st, "m1", [128, NLT], F32)
                m2 = SB(st, "m2", [128, NLT], F32)
                ptr = [PS(st, "dptr%d" % i, [128, DM], BF16) for i in range(2)]
                ptr2 = PS(st, "dptr2", [128, DM], BF16)
                pmix = [PS(st, "pmix%d" % i, [128, DM], F32) for i in range(2)]
                pr = PS(st, "pr", [128, 512], F32)
                S.dma("sp", lambda e: e.dma_start(out=aog[:], in_=aog_d[:, :]), "aog", W=["aog"])
                S.dma("pool", lambda e: e.dma_start(out=Wr[:], in_=wr_d[:, :].rearrange("(c p) n -> p c n", p=128)), "Wr", W=["Wr"])
                S.dma("sp", lambda e: e.dma_start(out=brb[:], in_=br_d[0:1, :].partition_broadcast(128)), "brb", W=["brb"])
                S.op("dve", lambda e: e.memset(zt[:], 0.0), W=["zt"])
                S.dma("sp", lambda e: e.dma_start(out=h2s_d[SEQ:OUT_ROWS, :], in_=zt[:]), "zt", R=["zt"])
                for k in range(8):
                    S.dma("sp", lambda e: e.dma_start(out=stg[k % 2][:], in_=wout_d[k * 128:(k + 1) * 128, :]), "stg%d" % (k % 2), W=[("stg", k % 2)])
                    S.op("dve", lambda e: e.tensor_scalar(out=Wout[:, k, :], in0=stg[k % 2][:], scalar1=aog[:, k:k + 1], scalar2=None, op0=ALU.mult),
                         R=[("stg", k % 2), "aog"], W=["Wout"])
                ntl = NLT if (LIM[0] > NLT or stage != 4) else LIM[0]

                def stage_a(lt):
                    b2 = lt % 2
                    for k in range(8):
                        src = QO[:, lt // 4, lt % 4, k * 128:(k + 1) * 128] if k < 4 else OF[:, lt, (k - 4) * 128:(k - 3) * 128]
                        S.op("pe", lambda e: e.transpose(ptr[b2][:, k * 128:(k + 1) * 128], src, ident[:]), R=["ident"], W=[("dptr", b2)], signal=(k == 7))
                    S.op("act", lambda e: e.activation(out=catT[b2][:], in_=ptr[b2][:].rearrange("p (k t) -> p k t", k=8), func=AF.Copy), W=[("dptr", b2), ("catT", b2)])
                    for nh in range(2):
                        for k in range(8):
                            S.op("pe", lambda e: e.matmul(pmix[b2][:, nh * 512:(nh + 1) * 512], lhsT=catT[b2][:, k, :], rhs=Wout[:, k, nh * 512:(nh + 1) * 512], start=(k == 0), stop=(k == 7)),
                                 R=[("catT", b2), "Wout"], W=[("pmix", b2)], signal=(k == 7 and nh == 1))
                    S.dma("sp", lambda e: e.dma_start(out=xt[b2][:], in_=x_d[lt * 128:(lt + 1) * 128, :]), "dxt%d" % b2, W=[("dxt", b2)])
                stage_a(0)
                for lt in range(ntl):
                    b2 = lt % 2
                    S.op("dve", lambda e: e.tensor_tensor(out=tmp[:], in0=pmix[b2][:], in1=G1[:], op=ALU.mult), W=[("pmix", b2), "dtmp"])
                    S.op("pool", lambda e: e.tensor_add(x1[b2][:], tmp[:], xt[b2][:]), R=["dtmp", ("dxt", b2)], W=[("x1", b2)])
                    if lt + 1 < ntl:
                        stage_a(lt + 1)
                    S.dma("sp", lambda e: e.dma_start(out=out_d[lt * 128:(lt + 1) * 128, :], in_=x1[b2][:]), "x1o%d" % b2, R=[("x1", b2)])
                    S.op("act", lambda e: e.activation(out=tmp[:], in_=x1[b2][:], func=AF.Square, accum_out=ss[:]), R=[("x1", b2)], W=["dtmp", "dss"])
                    S.op("act", lambda e: e.activation(out=rs[:], in_=ss[:], func=AF.Sqrt, scale=1.0 / DM, bias=epst[:]), R=["dss", "eps"], W=["drs"])
                    S.op("dve", lambda e: e.reciprocal(rs[:], rs[:]), R=["drs"], W=["drs"])
                    S.op("dve", lambda e: e.scalar_tensor_tensor(out=tmp[:], in0=x1[b2][:], scalar=rs[:, 0:1], in1=A2[:], op0=ALU.mult, op1=ALU.mult),
                         R=[("x1", b2), "drs"], W=["dtmp"])
                    S.op("pool", lambda e: e.tensor_add(h2b[b2][:], tmp[:], B2[:]), R=["dtmp"], W=[("h2b", b2)])
                    S.dma("sp", lambda e: e.dma_start(out=h2s_d[lt * 128:(lt + 1) * 128, :], in_=h2b[b2][:]), "h2o%d" % b2, R=[("h2b", b2)])
                    for k in range(8):
                        S.op("pe", lambda e: e.transpose(ptr2[:, k * 128:(k + 1) * 128], h2b[b2][:, k * 128:(k + 1) * 128], ident[:]), R=[("h2b", b2), "ident"], W=["dptr2"], signal=(k == 7))
                    S.op("act", lambda e: e.activation(out=h2T[:], in_=ptr2[:].rearrange("p (k t) -> p k t", k=8), func=AF.Copy), W=["dptr2", "h2T"])
                    for k in range(8):
                        S.op("pe", lambda e: e.matmul(pr[:, 0:36], lhsT=h2T[:, k, :], rhs=Wr[:, k, :], start=(k == 0), stop=(k == 7)), R=["h2T", "Wr"], W=["pr"], signal=(k == 7))
                    S.op("dve", lambda e: e.tensor_add(LG[:, lt, :], pr[:, 0:36], brb[:]), R=["brb"], W=["pr", ("LG", lt)])
                T = NLT
                lgG = LG[:, :, 0:4]
                allLG = [("LG", t_) for t_ in range(NLT)]
                S.op("dve", lambda e: e.tensor_reduce(out=gmx[:], in_=lgG, axis=AX.X, op=ALU.max), R=allLG, W=["gmx"])
                S.op("dve", lambda e: e.tensor_tensor(out=ohg[:], in0=lgG, in1=gmx[:].unsqueeze(2).to_broadcast([128, T, 4]), op=ALU.is_equal), R=allLG + ["gmx"], W=["ohg"])
                S.op("dve", lambda e: e.tensor_tensor(out=eg[:], in0=lgG, in1=gmx[:].unsqueeze(2).to_broadcast([128, T, 4]), op=ALU.subtract), R=allLG + ["gmx"], W=["eg"])
                S.op("act", lambda e: e.activation(out=eg[:], in_=eg[:], func=AF.Exp), R=["eg"], W=["eg"])
                S.op("dve", lambda e: e.tensor_reduce(out=pgv[:], in_=eg[:], axis=AX.X, op=ALU.add), R=["eg"], W=["pgv"])
                S.op("dve", lambda e: e.reciprocal(pgv[:], pgv[:]), R=["pgv"], W=["pgv"])
                lex = LG[:, :, 4:36].rearrange("p t (g x) -> p t x g", g=4)
                S.op("dve", lambda e: e.tensor_tensor(out=prod[:], in0=lex, in1=ohg[:].unsqueeze(2).to_broadcast([128, T, 8, 4]), op=ALU.mult), R=allLG + ["ohg"], W=["prod"])
                S.op("dve", lambda e: e.tensor_reduce(out=les[:], in_=prod[:], axis=AX.X, op=ALU.add), R=["prod"], W=["les"])
                S.op("dve", lambda e: e.tensor_reduce(out=m1[:], in_=les[:], axis=AX.X, op=ALU.max), R=["les"], W=["m1"])
                S.op("dve", lambda e: e.tensor_tensor(out=oh1[:], in0=les[:], in1=m1[:].unsqueeze(2).to_broadcast([128, T, 8]), op=ALU.is_equal), R=["les", "m1"], W=["oh1"])
                S.op("dve", lambda e: e.scalar_tensor_tensor(out=les2[:], in0=oh1[:], scalar=-1e30, in1=les[:], op0=ALU.mult, op1=ALU.add), R=["oh1", "les"], W=["les2"])
                S.op("dve", lambda e: e.tensor_reduce(out=m2[:], in_=les2[:], axis=AX.X, op=ALU.max), R=["les2"], W=["m2"])
                S.op("dve", lambda e: e.tensor_tensor(out=oh2[:], in0=les2[:], in1=m2[:].unsqueeze(2).to_broadcast([128, T, 8]), op=ALU.is_equal), R=["les2", "m2"], W=["oh2"])
                S.op("dve", lambda e: e.tensor_sub(m1[:], m1[:], m2[:]), R=["m1", "m2"], W=["m1"])
                S.op("act", lambda e: e.activation(out=m2[:], in_=m1[:], func=AF.Sigmoid), R=["m1"], W=["m2"])
                S.op("dve", lambda e: e.tensor_mul(W12[:, :, 0], m2[:], pgv[:]), R=["m2", "pgv"], W=["W12a"])
                S.op("dve", lambda e: e.tensor_sub(W12[:, :, 1], pgv[:], W12[:, :, 0]), R=["pgv", "W12a"], W=["W12b"])
                S.op("dve", lambda e: e.tensor_tensor(out=M1[:].rearrange("p t (g x) -> p t g x", g=4), in0=ohg[:].unsqueeze(3).to_broadcast([128, T, 4, 8]),
                                                      in1=oh1[:].unsqueeze(2).to_broadcast([128, T, 4, 8]), op=ALU.mult), R=["ohg", "oh1"], W=["M1"])
                S.op("dve", lambda e: e.tensor_tensor(out=M2[:].rearrange("p t (g x) -> p t g x", g=4), in0=ohg[:].unsqueeze(3).to_broadcast([128, T, 4, 8]),
                                                      in1=oh2[:].unsqueeze(2).to_broadcast([128, T, 4, 8]), op=ALU.mult), R=["ohg", "oh2"], W=["M2"])
                S.op("dve", lambda e: e.tensor_add(Msum[:].rearrange("p t x -> p (t x)"), M1[:].rearrange("p t x -> p (t x)"), M2[:].rearrange("p t x -> p (t x)")), R=["M1", "M2"], W=["Msum"])
            S.barrier()

        if stage >= 4:
            phase_d()
            if dbg and stage == 4:
                S.dma("sp", lambda e: e.dma_start(out=DO("d_M1", [128, NLT * 32])[:, :], in_=M1[:].rearrange("p a b -> p (a b)")), "o0")
                S.dma("sp", lambda e: e.dma_start(out=DO("d_M2", [128, NLT * 32])[:, :], in_=M2[:].rearrange("p a b -> p (a b)")), "o1")
                S.dma("sp", lambda e: e.dma_start(out=DO("d_W12", [128, NLT * 2])[:, :], in_=W12[:].rearrange("p a b -> p (a b)")), "o2")
                S.barrier()


        def routing():
            with ExitStack() as st:
                cnt = SB(st, "cnt", [128, 32], F32)
                cmp3 = SB(st, "cmp3", [128, NBLK, 32], F32)
                nblk = SB(st, "nblk", [128, 32], F32)
                padded = SB(st, "padded", [128, 32], F32)
                pend = SB(st, "pend", [128, 32], F32)
                pstart = SB(st, "pstart", [128, 32], F32)
                ones32 = SB(st, "ones32", [128, 32], F32)
                blke = SB(st, "blke", [128, NBLK], F32)
                same = SB(st, "same", [128, NBLK], F32)
                slotx = SB(st, "slotx", [128, NBLK], F32)
                tokf = SB(st, "tokf", [128, 32], F32)
                slotf = SB(st, "slotf", [128, 32], F32)
                t1 = SB(st, "t1", [128, 32], F32)
                DEST = SB(st, "DEST", [128, NLT, 2], F32)
                DESTI = SB(st, "DESTI", [128, NLT, 2], I32)
                REC = SB(st, "REC", [128, NLT, 2, 2], F32)
                PRE = SB(st, "PRE", [128, NBLK, 2], F32)
                pref = SB(st, "pref", [128, 1], F32)
                pcnt = PS(st, "pcnt", [128, 512], F32)
                ppos = PS(st, "ppos", [128, 512], F32)
                RECi = REC[:].bitcast(I32)
                PREi = PRE[:].bitcast(I32)
                S.op("dve", lambda e: e.memset(PRE[:].rearrange("p a b -> p (a b)"), 0.0), W=["PRE"])
                S.op("dve", lambda e: e.tensor_scalar(out=pref[:], in0=iota_p, scalar1=float(SEQ), scalar2=None, op0=ALU.add), R=["cst"], W=["pref"])
                S.op("dve", lambda e: e.tensor_copy(PREi[:, :, 0], pref[:].to_broadcast([128, NBLK])), R=["pref", "PRE"], W=["PRE"])
                S.dma("sp", lambda e: e.dma_start(out=slots_d.rearrange("(j p) r -> p j r", p=128), in_=PREi), "pre", R=["PRE"], W=["slots"])
                S.op("dve", lambda e: e.memset(ones32[:], 1.0), W=["ones32"])
                for lt in range(NLT):
                    S.op("pe", lambda e: e.matmul(pcnt[:, 0:32], lhsT=ones_bf[:], rhs=Msum[:, lt, :], start=(lt == 0), stop=(lt == NLT - 1)),
                         R=["ones"], W=["pcnt"], signal=(lt == NLT - 1))
                S.op("dve", lambda e: e.tensor_copy(cnt[:], pcnt[:, 0:32]), W=["pcnt", "cnt"])
                S.op("dve", lambda e: e.tensor_tensor(out=cmp3[:, 0:32, :], in0=cnt[:].unsqueeze(2).to_broadcast([128, 32, 32]),
                                                      in1=blk128[:, 0:32].unsqueeze(1).to_broadcast([128, 32, 32]), op=ALU.is_gt), R=["cnt", "cst"], W=["cmp3"])
                S.op("dve", lambda e: e.tensor_reduce(out=nblk[:], in_=cmp3[:, 0:32, :], axis=AX.X, op=ALU.add), R=["cmp3"], W=["nblk"])
                S.op("dve", lambda e: e.tensor_scalar(out=padded[:], in0=nblk[:], scalar1=128.0, scalar2=None, op0=ALU.mult), R=["nblk"], W=["padded"])
                S.op("dve", lambda e: e.tensor_tensor_scan(out=pend[:], data0=ones32[:], data1=padded[:], initial=0.0, op0=ALU.mult, op1=ALU.add), R=["ones32", "padded"], W=["pend"])
                S.op("dve", lambda e: e.tensor_sub(pstart[:], pend[:], padded[:]), R=["pend", "padded"], W=["pstart"])
                S.op("dve", lambda e: e.tensor_tensor(out=cmp3[:], in0=blk128[:, 0:NBLK].unsqueeze(2).to_broadcast([128, NBLK, 32]),
                                                      in1=pend[:].unsqueeze(1).to_broadcast([128, NBLK, 32]), op=ALU.is_ge), R=["pend", "cst", "nblk"], W=["cmp3"])
                S.op("dve", lambda e: e.tensor_reduce(out=blke[:], in_=cmp3[:], axis=AX.X, op=ALU.add), R=["cmp3"], W=["blke"])
                S.op("dve", lambda e: e.tensor_scalar(out=blke[:], in0=blke[:], scalar1=31.0, scalar2=None, op0=ALU.min), R=["blke"], W=["blke"])
                S.op("dve", lambda e: e.memset(same[:], 0.0), W=["same"])
                S.op("dve", lambda e: e.tensor_tensor(out=same[:, 1:NBLK], in0=blke[:, 1:NBLK], in1=blke[:, 0:NBLK - 1], op=ALU.is_equal), R=["blke", "same"], W=["same"])
                S.op("dve", lambda e: e.tensor_scalar(out=blke[:], in0=blke[:], scalar1=128.0, scalar2=None, op0=ALU.mult), R=["blke", "same"], W=["blke"])
                S.op("dve", lambda e: e.tensor_scalar(out=blke[:], in0=blke[:], scalar1=iota_p, scalar2=None, op0=ALU.add), R=["blke", "cst"], W=["blke"])
                S.op("dve", lambda e: e.tensor_copy(IDXW[:], blke[:]), R=["blke"], W=["IDXW"])
                S.op("dve", lambda e: e.scalar_tensor_tensor(out=same[:], in0=same[:], scalar=8192.0, in1=blke[:], op0=ALU.mult, op1=ALU.add), R=["blke", "same"], W=["same"])
                for hh in range(2):
                    S.op("dve", lambda e: e.tensor_scalar(out=slotx[:], in0=same[:], scalar1=2.0, scalar2=float(hh), op0=ALU.mult, op1=ALU.add), R=["same"], W=["slotx"])
                    S.op("dve", lambda e: e.tensor_copy(IDXS[:, hh, :], slotx[:]), R=["slotx"], W=["IDXS"])
                S.op("dve", lambda e: e.tensor_scalar(out=tokf[:], in0=blk128[:, 0:32], scalar1=iota_p, scalar2=None, op0=ALU.add), R=["cst"], W=["tokf"])
                for k in range(2):
                    S.op("dve", lambda e: e.tensor_copy(RECi[:, :, k, 0], tokf[:]), R=["tokf"], W=[("REC", k, 0)])
                    S.op("dve", lambda e: e.tensor_copy(REC[:, :, k, 1], W12[:, :, k]), W=[("REC", k, 1)])
                for lt in range(NLT):
                    for j in range(lt):
                        S.op("pe", lambda e: e.matmul(ppos[:, 0:32], lhsT=ones_bf[:], rhs=Msum[:, j, :], start=(j == 0), stop=False), R=["ones"], W=["ppos"], signal=False)
                    S.op("pe", lambda e: e.matmul(ppos[:, 0:32], lhsT=ustr[:], rhs=Msum[:, lt, :], start=(lt == 0), stop=True), R=["ustr"], W=["ppos"])
                    S.op("dve", lambda e: e.tensor_add(slotf[:], ppos[:, 0:32], pstart[:]), R=["pstart"], W=["ppos", "slotf"])
                    for k, Mk in enumerate((M1, M2)):
                        S.op("dve", lambda e: e.tensor_mul(t1[:], slotf[:], Mk[:, lt, :]), R=["slotf"], W=["t1"])
                        S.op("dve", lambda e: e.tensor_reduce(out=DEST[:, lt, k:k + 1], in_=t1[:], axis=AX.X, op=ALU.add), R=["t1"], W=[("DEST", lt, k)])
                S.op("dve", lambda e: e.tensor_copy(DESTI[:].rearrange("p a b -> p (a b)"), DEST[:].rearrange("p a b -> p (a b)")),
                     R=[("DEST", lt, k) for lt in range(NLT) for k in range(2)], W=["DESTI"])
                S.barrier()
                n = 0
                for lt in range(NLT):
                    for k in range(2):
                        S.dma("pool", lambda e: e.indirect_dma_start(out=slots_d[:, :], out_offset=bass.IndirectOffsetOnAxis(ap=DESTI[:, lt, k:k + 1], axis=0),
                                                                     in_=RECi[:, lt, k, :], in_offset=None), "sc%d" % (n % 4))
                        n += 1
                S.barrier()
                S.dma("sp", lambda e: e.dma_start(out=SLOT[:], in_=slots_d.rearrange("(j p) r -> p j r", p=128)), "slotrd", W=["SLOT"])
                S.barrier()

        def phase_e():
            with ExitStack() as st:
                Wg = [SB(st, "Wg0", [128, 8, 512], BF16)]
                Wu = [SB(st, "Wu0", [128, 8, 512], BF16)]
                Wd = [SB(st, "Wd0", [128, 4, DM], BF16)]
                xb = [SB(st, "xb%d" % i, [128, DM], BF16) for i in range(2)]
                xbT = SB(st, "xbT", [128, 8, 128], BF16)
                sil = SB(st, "sil", [128, 512], F32)
                atm = SB(st, "atm", [128, 512], BF16)
                aT = SB(st, "aT", [128, 4, 128], BF16)
                ys = [SB(st, "ys%d" % i, [128, DM], F32) for i in range(2)]
                ptr = PS(st, "eptr", [128, DM], BF16)
                pg = PS(st, "epg", [128, 512], F32)
                pu = PS(st, "epu", [128, 512], F32)
                paT = PS(st, "epaT", [128, DM], BF16)
                py = PS(st, "epy", [128, DM], F32)
                SLOTF = SLOT[:].bitcast(F32)
                nb = NBLK if (LIM[0] > NBLK or stage != 5) else LIM[0]

                bcreg = nc.gpsimd.alloc_register("bcreg")
                nc.gpsimd.reg_mov(bcreg, 8191)

                def gather(j):
                    b = j % 2
                    S.dma("pool", lambda e: e.indirect_dma_start(out=xb[b][:], out_offset=None, in_=h2s_d[:, :], in_offset=bass.IndirectOffsetOnAxis(ap=SLOT[:, j, 0:1], axis=0)),
                          "xb%d" % b, R=["SLOT"], W=[("xb", b)])

                def convert(j=None, which=(0, 1, 2)):
                    if j is None:
                        return
                    for nm, W_, src in [(("Wg", Wg[0], weg_d), ("Wu", Wu[0], weu_d), ("Wd", Wd[0], wed_d))[w_] for w_ in which]:
                        src2 = src.rearrange("r c n -> r (c n)").rearrange("r (h m) -> (r h) m", h=2)
                        dst2 = W_[:].rearrange("p c n -> p (c n)")
                        for hh in range(2):
                            ix = IDXS[:, hh, j:j + 1]
                            S.dma("pool", lambda e: e.indirect_dma_start(out=dst2[:, hh * 2048:(hh + 1) * 2048], out_offset=None, in_=src2,
                                                                         in_offset=bass.IndirectOffsetOnAxis(ap=ix, axis=0), bounds_check=bcreg, oob_is_err=False),
                                  "%s%d" % (nm, hh), R=["IDXS"], W=[(nm, hh)])
                gather(0)
                convert(0)
                for j in range(nb):
                    b = j % 2
                    if j + 1 < nb:
                        gather(j + 1)
                    for k in range(8):
                        S.op("pe", lambda e: e.transpose(ptr[:, k * 128:(k + 1) * 128], xb[b][:, k * 128:(k + 1) * 128], ident[:]), R=[("xb", b), "ident"], W=["eptr"], signal=(k == 7))
                    S.op("act", lambda e: e.activation(out=xbT[:], in_=ptr[:].rearrange("p (k t) -> p k t", k=8), func=AF.Copy), W=["eptr", "xbT"])
                    for k in range(8):
                        S.op("pe", lambda e: e.matmul(pg[:], lhsT=xbT[:, k, :], rhs=Wg[0][:, k, :], start=(k == 0), stop=(k == 7)), R=["xbT", ("Wg", 0), ("Wg", 1)], W=["epg"], signal=False)
                        S.op("pe", lambda e: e.matmul(pu[:], lhsT=xbT[:, k, :], rhs=Wu[0][:, k, :], start=(k == 0), stop=(k == 7)), R=["xbT", ("Wu", 0), ("Wu", 1)], W=["epu"], signal=(k == 7))
                    if j + 1 < nb:
                        convert(j + 1, which=(0, 1))
                    S.op("act", lambda e: e.activation(out=sil[:], in_=pg[:], func=AF.Silu), W=["epg", "sil"])
                    S.op("dve", lambda e: e.tensor_tensor(out=atm[:], in0=pu[:], in1=sil[:], op=ALU.mult), R=["sil"], W=["epu", "atm"])
                    for f in range(4):
                        S.op("pe", lambda e: e.transpose(paT[:, f * 128:(f + 1) * 128], atm[:, f * 128:(f + 1) * 128], ident[:]), R=["atm", "ident"], W=["epaT"], signal=(f == 3))
                    S.op("act", lambda e: e.activation(out=aT[:], in_=paT[:, 0:512].rearrange("p (f t) -> p f t", f=4), func=AF.Copy), W=["epaT", "aT"])
                    for nh in range(2):
                        for f in range(4):
                            S.op("pe", lambda e: e.matmul(py[:, nh * 512:(nh + 1) * 512], lhsT=aT[:, f, :], rhs=Wd[0][:, f, nh * 512:(nh + 1) * 512], start=(f == 0), stop=(f == 3)),
                                 R=["aT", ("Wd", 0), ("Wd", 1)], W=["epy"], signal=(f == 3 and nh == 1))
                    S.op("dve", lambda e: e.scalar_tensor_tensor(out=ys[b][:], in0=py[:], scalar=SLOTF[:, j, 1:2], in1=G2[:], op0=ALU.mult, op1=ALU.mult),
                         R=["SLOT"], W=["epy", ("ys", b)])
                    if j + 1 < nb:
                        convert(j + 1, which=(2,))
                    if LIM[1] & 8:
                        S.dma("sp", lambda e: e.dma_start(out=DO("d_ys%d" % j, [128, DM])[:, :], in_=ys[b][:]), "dys%d" % b, R=[("ys", b)])
                        continue
                    S.dma("pool", lambda e: e.indirect_dma_start(out=out_d[:, :], out_offset=bass.IndirectOffsetOnAxis(ap=SLOT[:, j, 0:1], axis=0),
                                                                 in_=ys[b][:], in_offset=None, compute_op=ALU.add), "scat", R=[("ys", b), "SLOT"], W=["outd"])
            S.barrier()

        if stage >= 5:
            routing()
            if dbg and stage == 5:
                S.dma("sp", lambda e: e.dma_start(out=DO("d_SLOT", [128, NBLK * 2], I32)[:, :], in_=SLOT[:].rearrange("p a b -> p (a b)")), "o0")
                S.dma("sp", lambda e: e.dma_start(out=DO("d_IDXW", [128, NBLK], I32)[:, :], in_=IDXW[:]), "o1")
                S.barrier()
            if not (LIM[1] & 4):
                phase_e()

        if dbg and stage == 0:
            S.dma("sp", lambda e: e.dma_start(out=DO("d_A1", [128, DM])[:, :], in_=A1[:]), "o0", R=["A1"])
            S.dma("sp", lambda e: e.dma_start(out=DO("d_B1", [128, DM])[:, :], in_=B1[:]), "o1", R=["B1"])
            S.dma("sp", lambda e: e.dma_start(out=DO("d_cA1", [128, DM])[:, :], in_=cA1[:]), "o2", R=["cA1"])
            S.dma("sp", lambda e: e.dma_start(out=DO("d_lb", [128, 8])[:, :], in_=lbv[:]), "o3", R=["lbv"])
        S.barrier()
    return nc, dbg_d


def host_consts():
    c = np.zeros((128, 1152), np.float32)
    c[:, 0:128] = np.eye(128)
    s = np.arange(128)[:, None]
    t = np.arange(128)[None, :]
    c[:, 128:256] = (s <= t)
    c[:, 256:384] = (s >= t)
    seg = np.ones(256, np.float32)
    seg[0::64] = 0
    c[:, 384:640] = seg[None, :]
    c[:, 640] = np.arange(128)
    same = (s // 64) == (t // 64)
    c[:, 896:1024] = (s <= t) & same
    c[:, 1024:1152] = (s >= t) & same
    c[:, 656:752] = (np.arange(96) * 128)[None, :]
    half = 32
    freqs = (10000.0 ** (-np.arange(0, half, 2, dtype=np.float32) / half)).astype(np.float32)
    tok = np.arange(SEQ)
    row = (tok // 64).astype(np.float32)
    col = (tok % 64).astype(np.float32)
    ang = np.stack([row[:, None] * freqs, col[:, None] * freqs], axis=1).astype(np.float32)
    cos = np.cos(ang).reshape(SEQ, 32)
    sin = np.sin(ang).reshape(SEQ, 32)
    r = np.concatenate([cos, sin], axis=1).reshape(NLT, 128, 64).transpose(1, 0, 2).reshape(128, NLT * 64)
    return c, np.ascontiguousarray(r.astype(np.float32))


def make_in_maps(inp):
    f = lambda a: np.ascontiguousarray(np.asarray(a, dtype=np.float32))
    cst, rope = host_consts()
    c_ctx = f(inp["c_ctx"]).reshape(8, 128).T
    qkg = np.concatenate([np.tile(f(inp["q_norm_g"])[0], 8), np.tile(f(inp["k_norm_g"])[0], 2)])[None, :]
    aog = np.concatenate([f(inp["attn_out_g"])[0], f(inp["hgrn_out_g"])[0]]).reshape(8, 128).T
    lb = f(inp["hgrn_lb"])
    lbt = lb.reshape(2, 2, 4, 128).transpose(3, 0, 1, 2).reshape(128, 16)
    wr = np.concatenate([f(inp["w_router_grp"])[0], f(inp["w_router_exp"])[0]], axis=1)
    br = np.concatenate([f(inp["b_router_grp"])[0], f(inp["b_router_exp"])[0]])[None, :]
    weg = f(inp["w_exp_gate"])[0].reshape(32, 8, 128, 512).transpose(0, 2, 1, 3).reshape(4096, 8, 512)
    weu = f(inp["w_exp_up"])[0].reshape(32, 8, 128, 512).transpose(0, 2, 1, 3).reshape(4096, 8, 512)
    wed = f(inp["w_exp_down"])[0].reshape(32, 4, 128, 1024).transpose(0, 2, 1, 3).reshape(4096, 4, 1024)
    shared = dict(
        w_ada=f(inp["w_ada"])[0], b_ada=f(inp["b_ada"]), n1g=f(inp["norm1_g"]), n2g=f(inp["norm2_g"]),
        w_in=f(inp["w_in"])[0], qkg=f(qkg), aog=f(aog), lbt=f(lbt), w_out=f(inp["w_out"])[0],
        wr=f(wr), br=f(br), weg=f(weg), weu=f(weu), wed=f(wed), cst=cst, rope=rope)
    maps = []
    for b in range(NCORES):
        cv = np.concatenate([f(inp["c"])[b].reshape(8, 128).T, c_ctx], axis=1)
        m = dict(shared)
        m.update(x=f(inp["x"])[b], ctx=f(inp["ctx"])[b], cvec=f(cv))
        maps.append(m)
    return maps


def kernel(**inputs):
    nc, _ = build()
    maps = make_in_maps(inputs)
    res = run_bass_kernel_spmd(nc, maps, core_ids=list(range(NCORES)))
    out = np.stack([np.asarray(r["out"])[:SEQ] for r in res.results], axis=0)
    return out.astype(np.float32)
```

```python
import numpy as np
from contextlib import ExitStack
import concourse.bass as bass
import concourse.mybir as mybir
from concourse.bass_utils import run_bass_kernel_spmd

F32 = mybir.dt.float32
BF16 = mybir.dt.bfloat16
I32 = mybir.dt.int32
AF = mybir.ActivationFunctionType
ALU = mybir.AluOpType
AX = mybir.AxisListType

NCORES = 8
LIM = [99, 0]
DM = 1024
SEQ = 4096
CTX = 256
TT = SEQ + CTX
NT = TT // 128
NLT = SEQ // 128
EPS = 1e-6
NBLK = 96
CAP = NBLK * 128
OUT_ROWS = SEQ + 128


class Sched:
    def __init__(self, nc, stack):
        self.nc = nc
        self.stack = stack
        self.eng = {"pe": nc.tensor, "act": nc.scalar, "dve": nc.vector, "pool": nc.gpsimd, "sp": nc.sync}
        self.sem = {e: stack.enter_context(nc.semaphore("s_" + e)) for e in self.eng}
        self.cnt = {e: 0 for e in self.eng}
        self.seen = {e: {} for e in self.eng}
        self.dsem = {}
        self.last_w = {}
        self.readers = {}
        self.nins = 0
        self.defer = None
        self.atom = None

    def _semof(self, key):
        if isinstance(key, tuple):
            return self.dsem[key[1]][0]
        return self.sem[key]

    def _deps(self, e, R, W):
        deps = {}

        def need(k, v):
            if v > deps.get(k, 0):
                deps[k] = v
        for b in R:
            w = self.last_w.get(b)
            if w:
                need(*w)
        for b in W:
            w = self.last_w.get(b)
            if w and (w[0] != e or e != "pe"):
                need(*w)
            for r in self.readers.get(b, ()):
                if r[0] != e or e != "pe":
                    need(*r)
        for k, v in deps.items():
            if self.seen[e].get(k, 0) >= v:
                continue
            if k == e:
                if v > self.cnt[e]:
                    continue
            self.eng[e].wait_ge(self._semof(k), v)
            self.seen[e][k] = v

    def op(self, e, fn, R=(), W=(), signal=True):
        if self.defer is not None:
            name, a, k = self._record(fn)
            R, W = list(R), list(W)
            tgt_list = self.atom if self.atom is not None else self.defer
            tgt_list.append(lambda: self.op(e, lambda eng: getattr(eng, name)(*a, **k), R, W, True))
            return None
        self._deps(e, R, W)
        ins = fn(self.eng[e])
        self.nins += 1
        tgt = self.cnt[e] + 1
        if signal:
            ins.then_inc(self.sem[e], 1)
            self.cnt[e] = tgt
        for b in R:
            self.readers.setdefault(b, []).append((e, tgt))
        for b in W:
            self.last_w[b] = (e, tgt)
            self.readers[b] = []
        return ins

    @staticmethod
    def _record(fn):
        class _Rec:
            def __getattr__(self, name):
                def f(*a, **k):
                    self.call = (name, a, k)
                return f
        r = _Rec()
        fn(r)
        return r.call

    def atomic_begin(self):
        if self.defer is not None:
            self.atom = []

    def atomic_end(self):
        if self.defer is not None and self.atom is not None:
            lst, self.atom = self.atom, None
            self.defer.append(lambda: [t() for t in lst])

    def flush(self, pend, n=None):
        n = len(pend) if n is None else min(n, len(pend))
        for _ in range(n):
            pend.pop(0)()

    def dma(self, q, fn, skey, R=(), W=()):
        if self.defer is not None:
            name, a, k = self._record(fn)
            R, W = list(R), list(W)
            self.defer.append(lambda: self.dma(q, lambda eng: getattr(eng, name)(*a, **k), skey, R, W))
            return None
        if skey not in self.dsem:
            self.dsem[skey] = [self.stack.enter_context(self.nc.semaphore("d_" + str(skey))), 0]
        ds = self.dsem[skey]
        key = ("d", skey)
        if ds[1] > 0 and self.seen[q].get(key, 0) < ds[1]:
            self.eng[q].wait_ge(ds[0], ds[1])
            self.seen[q][key] = ds[1]
        self._deps(q, R, W)
        ins = fn(self.eng[q])
        self.nins += 1
        ds[1] += 16
        ins.then_inc(ds[0], 16)
        for b in R:
            self.readers.setdefault(b, []).append((key, ds[1]))
        for b in W:
            self.last_w[b] = (key, ds[1])
            self.readers[b] = []
        return ins

    def barrier(self):
        for e in self.eng:
            for o in self.eng:
                if o != e and self.cnt[o] > self.seen[e].get(o, 0):
                    self.eng[e].wait_ge(self.sem[o], self.cnt[o])
                    self.seen[e][o] = self.cnt[o]
            for sk, (s, c) in self.dsem.items():
                k = ("d", sk)
                if c > self.seen[e].get(k, 0):
                    self.eng[e].wait_ge(s, c)
                    self.seen[e][k] = c
        self.last_w = {}
        self.readers = {}


def build(stage=99, dbg=False):
    nc = bass.Bass("TRN2", target_bir_lowering=False)

    def DI(name, shape, dt=F32):
        return nc.dram_tensor(name, shape, dt, kind="ExternalInput").ap()

    x_d = DI("x", [SEQ, DM])
    ctx_d = DI("ctx", [CTX, DM])
    cvec_d = DI("cvec", [128, 16])
    wada_d = DI("w_ada", [DM, 6 * DM])
    bada_d = DI("b_ada", [1, 6 * DM])
    n1g_d = DI("n1g", [1, DM])
    n2g_d = DI("n2g", [1, DM])
    win_d = DI("w_in", [DM, 3328])
    qkg_d = DI("qkg", [1, 640])
    aog_d = DI("aog", [128, 8])
    lbt_d = DI("lbt", [128, 16])
    wout_d = DI("w_out", [DM, DM])
    wr_d = DI("wr", [DM, 36])
    br_d = DI("br", [1, 36])
    weg_d = DI("weg", [4096, 8, 512])
    weu_d = DI("weu", [4096, 8, 512])
    wed_d = DI("wed", [4096, 4, 1024])
    cst_d = DI("cst", [128, 1152])
    rope_d = DI("rope", [128, NLT * 64])
    out_d = nc.dram_tensor("out", [OUT_ROWS, DM], F32, kind="ExternalOutput").ap()
    h2s_d = nc.dram_tensor("h2s", [OUT_ROWS, DM], BF16, kind="Internal").ap()
    slots_d = nc.dram_tensor("slots", [CAP, 2], I32, kind="Internal").ap()
    dbg_d = {}

    def DO(name, shape, dt=F32):
        dbg_d[name] = nc.dram_tensor(name, shape, dt, kind="ExternalOutput").ap()
        return dbg_d[name]

    with ExitStack() as top:
        S = Sched(nc, top)

        uid = [0]

        def SB(st, name, shape, dt):
            uid[0] += 1
            return st.enter_context(nc.sbuf_tensor("sb%d_%s" % (uid[0], name), shape, dt))

        def PS(st, name, shape, dt):
            uid[0] += 1
            return st.enter_context(nc.psum_tensor("ps%d_%s" % (uid[0], name), shape, dt))

        cst = SB(top, "cst", [128, 1152], F32)
        identf = cst[:, 0:128]
        maskU = cst[:, 128:256]
        maskL = cst[:, 256:384]
        segm = cst[:, 384:640]
        iota_p = cst[:, 640:641]
        blk128 = cst[:, 656:752]
        mU64 = cst[:, 896:1024]
        mL64 = cst[:, 1024:1152]
        ident = SB(top, "ident", [128, 128], BF16)
        ustr = SB(top, "ustr", [128, 128], BF16)
        ones_bf = SB(top, "ones_bf", [128, 128], BF16)
        epst = SB(top, "epst", [128, 1], F32)
        A1 = SB(top, "A1", [128, DM], F32)
        B1 = SB(top, "B1", [128, DM], F32)
        cA1 = SB(top, "cA1", [128, DM], F32)
        cB1 = SB(top, "cB1", [128, DM], F32)
        lbv = SB(top, "lbv", [128, 8], F32)
        omlb = SB(top, "omlb", [128, 8], F32)
        OF = SB(top, "OF", [128, NLT, 512], BF16)

        S.dma("sp", lambda e: e.dma_start(out=cst[:], in_=cst_d[:, :]), "cst", W=["cst"])
        S.op("dve", lambda e: e.tensor_copy(ident[:], identf), R=["cst"], W=["ident"])
        S.op("dve", lambda e: e.tensor_sub(ustr[:], maskU, identf), R=["cst"], W=["ustr"])
        S.op("dve", lambda e: e.memset(ones_bf[:], 1.0), W=["ones"])
        S.op("dve", lambda e: e.memset(epst[:], EPS), W=["eps"])

        def adaln_phase(cols, outs, lat_only_from):
            with ExitStack() as st:
                cv = SB(st, "cv", [128, 16], F32)
                cs = SB(st, "cs", [128, 16], F32)
                Lb = SB(st, "Lb", [128, 16, 128], F32)
                wa = [SB(st, "wa%d" % i, [128, 8, 512], F32) for i in range(2)]
                bb = SB(st, "bb", [128, 512], F32)
                pa = [PS(st, "pa%d" % i, [128, 512], F32) for i in range(2)]
                S.dma("sp", lambda e: e.dma_start(out=cv[:], in_=cvec_d[:, :]), "cv", W=["cv"])
                S.op("act", lambda e: e.activation(out=cs[:], in_=cv[:], func=AF.Silu), R=["cv"], W=["cs"])
                for j in range(16):
                    S.op("dve", lambda e, j=j: e.tensor_copy(Lb[:, j, :], cs[:, j:j + 1].to_broadcast([128, 128])),
                         R=["cs"], W=[("Lb", j)])
                for ci, n in enumerate(cols):
                    w = wa[ci % 2]
                    S.dma("sp", lambda e, w=w, n=n: e.dma_start(
                        out=w[:], in_=wada_d[:, n * 512:(n + 1) * 512].rearrange("(c p) n -> p c n", p=128)),
                        "wa%d" % (ci % 2), W=[("wa", ci % 2)])
                    S.dma("sp", lambda e, n=n: e.dma_start(
                        out=bb[:], in_=bada_d[0:1, n * 512:(n + 1) * 512].partition_broadcast(128)), "bb", W=["bb"])
                    for which in range(2):
                        dst = outs[n][which]
                        if dst is None:
                            continue
                        p = pa[which]
                        for k in range(8):
                            S.op("pe", lambda e, p=p, k=k, w=w, which=which: e.matmul(
                                p[:], lhsT=Lb[:, which * 8 + k, :], rhs=w[:, k, :], start=(k == 0), stop=(k == 7)),
                                R=[("Lb", which * 8 + k), ("wa", ci % 2)], W=[("pa", which)], signal=(k == 7))
                        S.op("dve", lambda e, p=p, dst=dst: e.tensor_add(dst, p[:], bb[:]),
                             R=["bb"], W=[("pa", which), ("mod", id(dst))])
            S.barrier()

        sh1 = B1
        with ExitStack() as st0:
            sc1 = SB(st0, "sc1", [128, DM], F32)
            csc1 = SB(st0, "csc1", [128, DM], F32)
            g1 = SB(st0, "g1", [128, DM], F32)
            outs = {0: (B1[:, 0:512], cB1[:, 0:512]), 1: (B1[:, 512:1024], cB1[:, 512:1024]),
                    2: (sc1[:, 0:512], csc1[:, 0:512]), 3: (sc1[:, 512:1024], csc1[:, 512:1024])}
            adaln_phase([0, 1, 2, 3], outs, None)
            S.dma("sp", lambda e: e.dma_start(out=g1[:], in_=n1g_d[0:1, :].partition_broadcast(128)), "g1", W=["g1"])
            S.op("dve", lambda e: e.scalar_tensor_tensor(out=A1[:], in0=sc1[:], scalar=1.0, in1=g1[:], op0=ALU.add, op1=ALU.mult),
                 R=["g1"], W=["A1"])
            S.op("dve", lambda e: e.scalar_tensor_tensor(out=cA1[:], in0=csc1[:], scalar=1.0, in1=g1[:], op0=ALU.add, op1=ALU.mult),
                 R=["g1"], W=["cA1"])
            lraw = SB(st0, "lraw", [128, 16], F32)
            S.dma("sp", lambda e: e.dma_start(out=lraw[:], in_=lbt_d[:, :]), "lraw", W=["lraw"])
            lr = lraw[:].rearrange("p (d l h) -> p d l h", d=2, l=2)
            ld = SB(st0, "ld", [128, 2, 4], F32)
            S.op("dve", lambda e: e.tensor_sub(ld[:], lr[:, :, 0, :], lr[:, :, 1, :]), R=["lraw"], W=["ld"])
            S.op("act", lambda e: e.activation(out=lbv[:].rearrange("p (d h) -> p d h", d=2), in_=ld[:], func=AF.Sigmoid),
                 R=["ld"], W=["lbv"])
            S.op("dve", lambda e: e.tensor_scalar(out=omlb[:], in0=lbv[:], scalar1=-1.0, scalar2=1.0, op0=ALU.mult, op1=ALU.add),
                 R=["lbv"], W=["omlb"])
            S.barrier()


        QO = SB(top, "QO", [128, 8, 4, 512], BF16)
        kvst = ExitStack()
        KTd = SB(kvst, "KTd", [128, 2, TT], BF16)
        Vaug = SB(kvst, "Vaug", [128, NT, 2, 66], BF16)
        S.op("dve", lambda e: e.memset(Vaug[:].rearrange("p a b c -> p (a b c)"), 1.0), W=["vones"])

        def pass_fb(dirn):
            with ExitStack() as st:
                Wq = SB(st, "Wq", [128, 8, 512], BF16)
                Wf = SB(st, "Wf", [128, 8, 512], BF16)
                Wi = SB(st, "Wi", [128, 8, 512], BF16)
                Wx = SB(st, "Wx", [128, 8, 768], BF16)
                fcol = 1280 if dirn == 0 else 1792
                for (w, c0, n, nm) in ((Wq, 768, 512, "Wq"), (Wf, fcol, 512, "Wf"), (Wi, 2304, 512, "Wi"),
                                      (Wx, 0 if dirn == 0 else 2816, 768 if dirn == 0 else 512, "Wx")):
                    S.dma("pool", lambda e, w=w, c0=c0, n=n: e.dma_start(
                        out=w[:, :, 0:n], in_=win_d[:, c0:c0 + n].rearrange("(c p) n -> p c n", p=128)), nm, W=[nm])
                if dirn == 0:
                    ropet = SB(st, "ropet", [128, NLT, 64], F32)
                    qkgain = SB(st, "qkgain", [128, 640], F32)
                    S.dma("sp", lambda e: e.dma_start(out=ropet[:].rearrange("p t c -> p (t c)"), in_=rope_d[:, :]), "ropet", W=["ropet"])
                    S.dma("sp", lambda e: e.dma_start(out=qkgain[:], in_=qkg_d[0:1, :].partition_broadcast(128)), "qkgain", W=["qkgain"])
                xt = [SB(st, "xt%d" % i, [128, DM], F32) for i in range(2)]
                tmp = SB(st, "tmp", [128, DM], F32)
                tmp_alias = tmp
                junk = tmp
                hb = SB(st, "hb", [128, DM], BF16)
                hTc = [SB(st, "hTc%d" % i, [128, 8, 2, 128], BF16) for i in range(2)]
                ss = SB(st, "ss", [128, 2], F32)
                rs = SB(st, "rs", [128, 2], F32)
                itm = [SB(st, "itm%d" % i, [128, 2, 512], BF16) for i in range(2)]
                if dirn == 1:
                    sg = [SB(st, "sg%d" % i, [128, 2, 512], BF16) for i in range(2)]
                    osum = SB(st, "osum", [128, 2, 512], F32)
                    hc = SB(st, "hc", [128, 256], F32)
                else:
                    sqb = tmp_alias
                    qn = SB(st, "qn", [128, 640], F32)
                    ss10 = SB(st, "ss10", [128, 10], F32)
                    rs10 = SB(st, "rs10", [128, 10], F32)
                    rt = [SB(st, "rt%d" % i, [128, 10, 32], F32) for i in range(2)] * 2
                    qkr = SB(st, "qkr", [128, 640], BF16)
                    kd2 = SB(st, "kd2", [128, 2, 128], BF16)
                hq = SB(st, "hq", [128, 256], F32)
                hf = SB(st, "hf", [128, 256], F32)
                hg = SB(st, "hg", [128, 256], F32)
                hk = SB(st, "hk", [128, 256], F32)
                hbb = SB(st, "hbb", [128, 256], F32)
                he = SB(st, "he", [128, 256], F32)
                ke = [SB(st, "ke%d" % i, [128, 256], BF16) for i in range(2)]
                Eend = [SB(st, "Eend%d" % i, [128, 4], F32) for i in range(2)]
                kdtm4 = SB(st, "kdtm4", [128, 4, 128], BF16)
                AT2 = SB(st, "AT2", [128, 2, 128], BF16)
                SB5 = SB(st, "SB5", [128, 5, 128], BF16)
                QEA = [SB(st, "QEA%d" % i, [128, 2, 128], BF16) for i in range(2)]
                QEB = [SB(st, "QEB%d" % i, [128, 2, 128], BF16) for i in range(2)]
                KDA = [SB(st, "KDA%d" % i, [128, 2, 128], BF16) for i in range(2)]
                KDB = [SB(st, "KDB%d" % i, [128, 2, 128], BF16) for i in range(2)]
                S32 = SB(st, "S32", [128, 4, 128], F32)
                ss4 = SB(st, "ss4", [128, 4], F32)
                rs4 = SB(st, "rs4", [128, 4], F32)
                ptr = PS(st, "ptr", [128, DM], BF16)
                psq = PS(st, "psq", [128, 512], F32)
                pskv = PS(st, "pskv", [128, 512], F32)
                psi = PS(st, "psi", [128, 512], F32)
                pfa = PS(st, "pfa", [128, 512], F32)
                pfb = PS(st, "pfb", [128, 512], F32)
                psh = PS(st, "psh", [128, 512], F32)
                pTb = PS(st, "pTb", [128, DM], BF16)
                S.op("dve", lambda e: e.memset(S32[:].rearrange("p a b -> p (a b)"), 0.0), W=["S32"])
                mask = mU64 if dirn == 0 else mL64
                endc = 63 if dirn == 0 else 0
                for nm_, t_ in (("QEA", QEA), ("QEB", QEB), ("KDA", KDA), ("KDB", KDB)):
                    for pb_ in range(2):
                        S.op("dve", lambda e, t_=t_, pb_=pb_: e.memset(t_[pb_][:].rearrange("p a b -> p (a b)"), 0.0), W=[(nm_, pb_)])
                chunks = list(range(17)) if dirn == 0 else [0] + list(range(16, 0, -1))
                if (dirn == 1 and stage == 2) or stage < 2:
                    chunks = chunks[:LIM[0]]
                def prologue(ci, cb):
                    for tl in range(2):
                        gt = ci * 2 + tl
                        isctx = gt < 2
                        lt = gt - 2
                        xs = xt[gt % 2]
                        src = ctx_d[gt * 128:(gt + 1) * 128, :] if isctx else x_d[lt * 128:(lt + 1) * 128, :]
                        S.dma("sp", lambda e, xs=xs, src=src: e.dma_start(out=xs[:], in_=src), "xt%d" % (gt % 2), W=[("xt", gt % 2)])
                        S.op("act", lambda e, xs=xs, tl=tl: e.activation(out=junk[:], in_=xs[:], func=AF.Square, accum_out=ss[:, tl:tl + 1]),
                             R=[("xt", gt % 2)], W=["tmp", ("ss", tl)])
                        S.op("act", lambda e, tl=tl: e.activation(out=rs[:, tl:tl + 1], in_=ss[:, tl:tl + 1], func=AF.Sqrt, scale=1.0 / DM, bias=epst[:]),
                             R=[("ss", tl), "eps"], W=[("rs", tl)])
                        S.op("dve", lambda e, tl=tl: e.reciprocal(rs[:, tl:tl + 1], rs[:, tl:tl + 1]), R=[("rs", tl)], W=[("rs", tl)])
                        Am, Bm = (cA1, cB1) if isctx else (A1, B1)
                        S.op("dve", lambda e, xs=xs, tl=tl, Am=Am: e.scalar_tensor_tensor(
                            out=tmp[:], in0=xs[:], scalar=rs[:, tl:tl + 1], in1=Am[:], op0=ALU.mult, op1=ALU.mult),
                            R=[("xt", gt % 2), ("rs", tl)], W=["tmp"])
                        S.op("pool", lambda e, Bm=Bm: e.tensor_add(hb[:], tmp[:], Bm[:]), R=["tmp"], W=["hb"])
                        for k in range(8):
                            S.op("pe", lambda e, k=k: e.transpose(ptr[:, k * 128:(k + 1) * 128], hb[:, k * 128:(k + 1) * 128], ident[:]),
                                 R=["hb", "ident"], W=["ptr"], signal=(k == 7))
                        S.op("act", lambda e, tl=tl: e.activation(out=hTc[cb][:, :, tl, :], in_=ptr[:].rearrange("p (k t) -> p k t", k=8), func=AF.Copy),
                             W=["ptr", ("hTc", cb, tl)])
                        for k in range(8):
                            lh = hTc[cb][:, k, tl, :]
                            if dirn == 0:
                                if not isctx:
                                    S.op("pe", lambda e, k=k, lh=lh: e.matmul(psq[:], lhsT=lh, rhs=Wx[:, k, 0:512], start=(k == 0), stop=(k == 7)),
                                         R=[("hTc", cb, tl), "Wx"], W=["psq"], signal=False)
                                S.op("pe", lambda e, k=k, lh=lh: e.matmul(pskv[:, 0:256], lhsT=lh, rhs=Wx[:, k, 512:768], start=(k == 0), stop=(k == 7)),
                                     R=[("hTc", cb, tl), "Wx"], W=["pskv"], signal=False)
                            elif not isctx:
                                S.op("pe", lambda e, k=k, lh=lh: e.matmul(pskv[:], lhsT=lh, rhs=Wx[:, k, 0:512], start=(k == 0), stop=(k == 7)),
                                     R=[("hTc", cb, tl), "Wx"], W=["pskv"], signal=False)
                            S.op("pe", lambda e, k=k, lh=lh: e.matmul(psi[:], lhsT=lh, rhs=Wi[:, k, :], start=(k == 0), stop=(k == 7)),
                                 R=[("hTc", cb, tl), "Wi"], W=["psi"], signal=(k == 7))
                        S.op("act", lambda e, tl=tl: e.activation(out=itm[cb][:, tl, :], in_=psi[:], func=AF.Copy), W=["psi", ("itm", cb, tl)])
                        if dirn == 1 and not isctx:
                            S.op("act", lambda e, tl=tl: e.activation(out=sg[cb][:, tl, :], in_=pskv[:], func=AF.Silu), W=["pskv", ("sg", cb, tl)])
                        if dirn == 0 and not (LIM[1] & 1):
                            S.op("act", lambda e, gt=gt: e.activation(out=Vaug[:, gt, :, 0:64], in_=pskv[:, 128:256].rearrange("p (a d) -> p a d", a=2), func=AF.Copy),
                                 W=["pskv", ("V", gt)])
                            nh = 2 if isctx else 10
                            o0 = 512 if isctx else 0
                            if not isctx:
                                S.op("act", lambda e: e.activation(out=sqb[:, 0:512], in_=psq[:], func=AF.Square), W=["psq", "sqbq", "tmp"])
                            S.op("act", lambda e: e.activation(out=sqb[:, 512:640], in_=pskv[:, 0:128], func=AF.Square), W=["pskv", "sqbk", "tmp"])
                            S.op("dve", lambda e, o0=o0: e.tensor_reduce(out=ss10[:, o0 // 64:10], in_=sqb[:, o0:640].rearrange("p (h d) -> p h d", d=64), axis=AX.X, op=ALU.add),
                                 R=["sqbq", "sqbk", "tmp"], W=["ss10"])
                            S.op("act", lambda e, o0=o0: e.activation(out=rs10[:, o0 // 64:10], in_=ss10[:, o0 // 64:10], func=AF.Sqrt, scale=1.0 / 64, bias=epst[:]),
                                 R=["ss10", "eps"], W=["rs10"])
                            S.op("dve", lambda e, o0=o0: e.reciprocal(rs10[:, o0 // 64:10], rs10[:, o0 // 64:10]), R=["rs10"], W=["rs10"])
                            if not isctx:
                                S.op("dve", lambda e: e.tensor_tensor(out=qn[:, 0:512].rearrange("p (h d) -> p h d", d=64), in0=psq[:].rearrange("p (h d) -> p h d", d=64),
                                                                      in1=rs10[:, 0:8].unsqueeze(2).to_broadcast([128, 8, 64]), op=ALU.mult),
                                     R=["rs10"], W=["psq", "qnq"])
                            S.op("dve", lambda e: e.tensor_tensor(out=qn[:, 512:640].rearrange("p (h d) -> p h d", d=64), in0=pskv[:, 0:128].rearrange("p (h d) -> p h d", d=64),
                                                                  in1=rs10[:, 8:10].unsqueeze(2).to_broadcast([128, 2, 64]), op=ALU.mult),
                                 R=["rs10"], W=["pskv", "qnk"])
                            S.op("pool", lambda e, o0=o0: e.tensor_mul(qn[:, o0:640], qn[:, o0:640], qkgain[:, o0:640]), R=["qnq", "qnk", "qkgain"], W=["qn"])
                            if isctx:
                                S.op("dve", lambda e: e.tensor_copy(kd2[:, :, 0:64], qn[:, 512:640].rearrange("p (h d) -> p h d", d=64)), R=["qn"], W=["kd2a"])
                                S.op("dve", lambda e: e.tensor_copy(kd2[:, :, 64:128], qn[:, 512:640].rearrange("p (h d) -> p h d", d=64)), R=["qn"], W=["kd2b"])
                            else:
                                qv = qn[:].rearrange("p (h c two) -> p h c two", h=10, two=2)
                                x1, x2 = qv[:, :, :, 0], qv[:, :, :, 1]
                                cosb = ropet[:, lt, 0:32].unsqueeze(1).to_broadcast([128, 10, 32])
                                sinb = ropet[:, lt, 32:64].unsqueeze(1).to_broadcast([128, 10, 32])
                                S.op("dve", lambda e: e.tensor_tensor(out=rt[0][:], in0=x1, in1=cosb, op=ALU.mult), R=["qn", "ropet"], W=["rt0"])
                                S.op("dve", lambda e: e.tensor_tensor(out=rt[1][:], in0=x2, in1=sinb, op=ALU.mult), R=["qn", "ropet"], W=["rt1"])
                                qo = qkr[:].rearrange("p (h c two) -> p h c two", h=10, two=2)
                                S.op("dve", lambda e: e.tensor_sub(qo[:, :, :, 0], rt[0][:], rt[1][:]), R=["rt0", "rt1"], W=["qkr0"])
                                S.op("dve", lambda e: e.tensor_tensor(out=rt[2][:], in0=x1, in1=sinb, op=ALU.mult), R=["qn", "ropet"], W=["rt0"])
                                S.op("dve", lambda e: e.tensor_tensor(out=rt[3][:], in0=x2, in1=cosb, op=ALU.mult), R=["qn", "ropet"], W=["rt1"])
                                S.op("dve", lambda e: e.tensor_add(qo[:, :, :, 1], rt[2][:], rt[3][:]), R=["rt0", "rt1"], W=["qkr1"])
                                S.op("dve", lambda e: e.tensor_copy(kd2[:, :, 0:64], qkr[:, 512:640].rearrange("p (h d) -> p h d", d=64)), R=["qkr0", "qkr1"], W=["kd2a"])
                                S.op("dve", lambda e: e.tensor_copy(kd2[:, :, 64:128], qkr[:, 512:640].rearrange("p (h d) -> p h d", d=64)), R=["qkr0", "qkr1"], W=["kd2b"])
                                for j in range(4):
                                    S.op("pe", lambda e, j=j: e.transpose(pTb[:, j * 128:(j + 1) * 128], qkr[:, j * 128:(j + 1) * 128], ident[:]),
                                         R=["qkr0", "qkr1", "ident"], W=["pTb"], signal=(j == 3))
                                S.op("act", lambda e, lt=lt: e.activation(out=QO[:, lt // 4, :, (lt % 4) * 128:(lt % 4 + 1) * 128], in_=pTb[:, 0:512].rearrange("p (j t) -> p j t", j=4), func=AF.Copy),
                                     W=["pTb", ("QO", lt // 4)])
                            for kv in range(2):
                                S.op("pe", lambda e, kv=kv: e.transpose(pTb[:, 512 + kv * 128:512 + (kv + 1) * 128], kd2[:, kv, :], ident[:]),
                                     R=["kd2a", "kd2b", "ident"], W=["pTb"], signal=(kv == 1))
                            S.op("act", lambda e, gt=gt: e.activation(out=KTd[:, :, gt * 128:(gt + 1) * 128], in_=pTb[:, 512:768].rearrange("p (j t) -> p j t", j=2), func=AF.Copy),
                                 W=["pTb", ("KT", gt)])
                def hgrn(ci, cb, pend):
                    pts = [44]

                    def fl():
                        if pend:
                            S.flush(pend, -(-len(pend) // max(pts[0], 1)))
                        pts[0] -= 1
                    def prep(hd, pb):
                        S.atomic_begin()
                        for k in range(8):
                            rh = hTc[cb][:, k, :, :].rearrange("p a t -> p (a t)")
                            S.op("pe", lambda e, k=k, rh=rh, hd=hd: e.matmul(pfa[:, 0:256], lhsT=Wq[:, k, hd * 128:(hd + 1) * 128], rhs=rh, start=(k == 0), stop=(k == 7)),
                                 R=[("hTc", cb, 0), ("hTc", cb, 1), "Wq"], W=["pfa"], signal=False)
                            S.op("pe", lambda e, k=k, rh=rh, hd=hd: e.matmul(pfb[:, 0:256], lhsT=Wf[:, k, hd * 128:(hd + 1) * 128], rhs=rh, start=(k == 0), stop=(k == 7)),
                                 R=[("hTc", cb, 0), ("hTc", cb, 1), "Wf"], W=["pfb"], signal=(k == 7))
                        S.atomic_end()
                        li = dirn * 4 + hd
                        S.op("act", lambda e: e.activation(out=hq[:], in_=pfa[:, 0:256], func=AF.Silu), W=["pfa", "hq"])
                        S.op("act", lambda e: e.activation(out=hf[:], in_=pfb[:, 0:256], func=AF.Sigmoid), W=["pfb", "hf"])
                        S.op("dve", lambda e, li=li: e.tensor_scalar(out=hf[:], in0=hf[:], scalar1=omlb[:, li:li + 1], scalar2=lbv[:, li:li + 1], op0=ALU.mult, op1=ALU.add),
                             R=["hf", "lbv", "omlb"], W=["hf"])
                        S.op("act", lambda e: e.activation(out=hg[:], in_=hf[:], func=AF.Ln), R=["hf"], W=["hg"])
                        if S.defer is None:
                            fl()
                        S.op("pool", lambda e: e.tensor_scalar(out=hk[:], in0=hf[:], scalar1=-1.0, scalar2=1.0, op0=ALU.mult, op1=ALU.add), R=["hf"], W=["hk"])
                        S.op("dve", lambda e: e.tensor_tensor_scan(out=hbb[:], data0=segm, data1=hg[:], initial=0.0, op0=ALU.mult, op1=ALU.add),
                             R=["hg", "cst"], W=["hbb"])
                        b4 = hbb[:].rearrange("p (c t) -> p c t", t=64)
                        if dirn == 0:
                            cum, cumk = hbb, "hbb"
                        else:
                            S.op("pool", lambda e: e.tensor_sub(hc[:], hg[:], hbb[:]), R=["hg", "hbb"], W=["hc"])
                            S.op("dve", lambda e: e.tensor_tensor(out=hc[:].rearrange("p (c t) -> p c t", t=64), in0=hc[:].rearrange("p (c t) -> p c t", t=64),
                                                                  in1=b4[:, :, 63:64].to_broadcast([128, 4, 64]), op=ALU.add), R=["hc", "hbb"], W=["hc"])
                            cum, cumk = hc, "hc"
                        c4 = cum[:].rearrange("p (c t) -> p c t", t=64)
                        if S.defer is None:
                            fl()
                        S.op("act", lambda e: e.activation(out=Eend[pb][:].unsqueeze(2), in_=c4[:, :, endc:endc + 1], func=AF.Exp), R=[cumk], W=[("Eend", pb)])
                        S.op("act", lambda e: e.activation(out=hg[:], in_=cum[:], func=AF.Exp), R=[cumk], W=["hg"])
                        hq4 = hq[:].rearrange("p (a c t) -> p a c t", a=2, c=2)
                        hg4 = hg[:].rearrange("p (a c t) -> p a c t", a=2, c=2)
                        S.op("dve", lambda e: e.scalar_tensor_tensor(out=QEA[pb][:, :, 0:64], in0=hq4[:, :, 0, :], scalar=float(128 ** -0.5), in1=hg4[:, :, 0, :], op0=ALU.mult, op1=ALU.mult),
                             R=["hq", "hg"], W=[("QEA", pb)])
                        S.op("dve", lambda e: e.scalar_tensor_tensor(out=QEB[pb][:, :, 64:128], in0=hq4[:, :, 1, :], scalar=float(128 ** -0.5), in1=hg4[:, :, 1, :], op0=ALU.mult, op1=ALU.mult),
                             R=["hq", "hg"], W=[("QEB", pb)])
                        if S.defer is None:
                            fl()
                        S.op("act", lambda e: e.activation(out=hf[:], in_=cum[:], func=AF.Exp, scale=-1.0), R=[cumk], W=["hf"])
                        S.op("pool", lambda e: e.tensor_mul(ke[pb][:], hk[:], hf[:]), R=["hk", "hf"], W=[("ke", pb)])
                        if S.defer is None:
                            fl()
                        S.op("dve", lambda e: e.tensor_tensor(out=he[:].rearrange("p (c t) -> p c t", t=64), in0=c4, in1=c4[:, :, endc:endc + 1].to_broadcast([128, 4, 64]), op=ALU.subtract),
                             R=[cumk], W=["he"])
                        S.op("act", lambda e: e.activation(out=hq[:], in_=he[:], func=AF.Exp, scale=-1.0), R=["he", ("QEA", pb), ("QEB", pb)], W=["hq"])
                        hk4 = hk[:].rearrange("p (a c t) -> p a c t", a=2, c=2)
                        S.op("dve", lambda e: e.tensor_tensor(out=KDA[pb][:, :, 0:64], in0=hk4[:, :, 0, :], in1=hq4[:, :, 0, :], op=ALU.mult), R=["hk", "hq"], W=[("KDA", pb)])
                        S.op("dve", lambda e: e.tensor_tensor(out=KDB[pb][:, :, 64:128], in0=hk4[:, :, 1, :], in1=hq4[:, :, 1, :], op=ALU.mult), R=["hk", "hq"], W=[("KDB", pb)])
                        if S.defer is None:
                            fl()
                    def phases(hd, pb, pend2):
                        pts2 = [6]

                        def fl2():
                            if pend2:
                                S.flush(pend2, -(-len(pend2) // max(pts2[0], 1)))
                            pts2[0] -= 1
                        hsl = slice(hd * 128, (hd + 1) * 128)
                        tls = (0, 1) if dirn == 0 else (1, 0)
                        cs = (0, 1) if dirn == 0 else (1, 0)
                        lat = ci > 0
                        lt0 = ci * 2 - 2
                        for q in range(4):
                            KDc, kk_ = (KDA[pb], ("KDA", pb)) if q % 2 == 0 else (KDB[pb], ("KDB", pb))
                            S.op("pe", lambda e: e.transpose(pTb[:, q * 128:(q + 1) * 128], KDc[:, q // 2, :], ident[:]), R=[kk_, "ident"], W=["pTb"], signal=(q == 3))
                        S.op("act", lambda e: e.activation(out=kdtm4[:].rearrange("p a b -> p (a b)"), in_=pTb[:, 0:512], func=AF.Copy), W=["pTb", "kdtm4"])
                        Ureg = [(pfa, 256, "pfa"), (pfa, 384, "pfa"), (pfb, 256, "pfb"), (pfb, 384, "pfb")]
                        for q in range(4):
                            ub, uo, uk = Ureg[q]
                            S.op("pe", lambda e: e.matmul(ub[:, uo:uo + 128], lhsT=kdtm4[:, q, :], rhs=itm[cb][:, q // 2, hsl], start=True, stop=True),
                                 R=["kdtm4", ("itm", cb, q // 2)], W=[uk], signal=(q % 2 == 1))
                        if lat:
                            for tl in range(2):
                                tsl = slice(tl * 128, (tl + 1) * 128)
                                S.op("pe", lambda e: e.matmul(psh[:, tsl], lhsT=ke[pb][:, tsl], rhs=QEA[pb][:, tl, :], start=True, stop=False), R=[("ke", pb), ("QEA", pb)], W=["psh"], signal=False)
                                S.op("pe", lambda e: e.matmul(psh[:, tsl], lhsT=ke[pb][:, tsl], rhs=QEB[pb][:, tl, :], start=False, stop=True), R=[("ke", pb), ("QEB", pb)], W=["psh"], signal=(tl == 1))
                            S.op("dve", lambda e: e.tensor_tensor(out=AT2[:], in0=psh[:, 0:256].rearrange("p (a t) -> p a t", a=2),
                                                                  in1=mask.unsqueeze(1).to_broadcast([128, 2, 128]), op=ALU.mult), R=["cst"], W=["psh", "AT2"])
                        fl()
                        fl2()
                        order = [(tl, c) for tl in tls for c in cs]
                        S.op("pool", lambda e: e.tensor_copy(SB5[:, 0, :], S32[:, hd, :]), R=["S32"], W=[("SB5", 0)])
                        for i, (tl, c) in enumerate(order):
                            q = tl * 2 + c
                            ub, uo, uk = Ureg[q]
                            S.op("dve", lambda e: e.scalar_tensor_tensor(out=SB5[:, i + 1, :], in0=S32[:, hd, :], scalar=Eend[pb][:, q:q + 1], in1=ub[:, uo:uo + 128], op0=ALU.mult, op1=ALU.add),
                                 R=["S32", ("Eend", pb)], W=[uk, ("SB5", i + 1)])
                            S.op("dve", lambda e: e.scalar_tensor_tensor(out=S32[:, hd, :], in0=S32[:, hd, :], scalar=Eend[pb][:, q:q + 1], in1=ub[:, uo:uo + 128], op0=ALU.mult, op1=ALU.add),
                                 R=[("Eend", pb)], W=[uk, "S32"])
                            fl()
                            fl2()
                        fl2()
                        if lat:
                            for tl in tls:
                                i0, i1 = order.index((tl, cs[0])), order.index((tl, cs[1]))
                                QE0, q0k = (QEA[pb], ("QEA", pb)) if cs[0] == 0 else (QEB[pb], ("QEB", pb))
                                QE1, q1k = (QEA[pb], ("QEA", pb)) if cs[1] == 0 else (QEB[pb], ("QEB", pb))
                                osl = slice(256 + tl * 128, 256 + (tl + 1) * 128)
                                S.op("pe", lambda e: e.matmul(psh[:, osl], lhsT=QE0[:, tl, :], rhs=SB5[:, i0, :], start=True, stop=False), R=[q0k, ("SB5", i0)], W=["psh"], signal=False)
                                S.op("pe", lambda e: e.matmul(psh[:, osl], lhsT=QE1[:, tl, :], rhs=SB5[:, i1, :], start=False, stop=False), R=[q1k, ("SB5", i1)], W=["psh"], signal=False)
                                S.op("pe", lambda e: e.matmul(psh[:, osl], lhsT=AT2[:, tl, :], rhs=itm[cb][:, tl, hsl], start=False, stop=True), R=["AT2", ("itm", cb, tl)], W=["psh"])
                            po3 = psh[:, 256:512].rearrange("p (a t) -> p a t", a=2)
                            if dirn == 0:
                                S.op("act", lambda e: e.activation(out=OF[:, lt0:lt0 + 2, hsl], in_=po3, func=AF.Copy), W=["psh", ("OF", lt0, hd), ("OF", lt0 + 1, hd)])
                            else:
                                S.op("dve", lambda e: e.tensor_tensor(out=osum[:, :, hsl], in0=po3, in1=OF[:, lt0:lt0 + 2, hsl], op=ALU.add),
                                     R=[("OF", lt0, hd), ("OF", lt0 + 1, hd)], W=["psh", ("osum", 0, hd), ("osum", 1, hd)])
                        fl()
                        fl2()
                        S.flush(pend2)
                    heads = list(range(4)) if not (LIM[1] & 2) else []
                    if heads:
                        prep(0, 0)
                    for hd in heads:
                        pend2 = []
                        if hd + 1 < 4:
                            S.defer = pend2
                            prep(hd + 1, (hd + 1) % 2)
                            S.defer = None
                        phases(hd, hd % 2, pend2)
                    if dirn == 1 and ci > 0:
                        for tl in range(2):
                            lt = ci * 2 + tl - 2
                            S.op("act", lambda e, tl=tl: e.activation(out=junk[:, 0:512], in_=osum[:, tl, :], func=AF.Square), R=[("osum", tl, h_) for h_ in range(4)], W=["tmp"])
                            S.op("dve", lambda e: e.tensor_reduce(out=ss4[:], in_=junk[:, 0:512].rearrange("p (h d) -> p h d", d=128), axis=AX.X, op=ALU.add), R=["tmp"], W=["ss4"])
                            S.op("act", lambda e: e.activation(out=rs4[:], in_=ss4[:], func=AF.Sqrt, scale=1.0 / 128, bias=epst[:]), R=["ss4", "eps"], W=["rs4"])
                            S.op("dve", lambda e: e.reciprocal(rs4[:], rs4[:]), R=["rs4"], W=["rs4"])
                            S.op("dve", lambda e, tl=tl: e.tensor_tensor(out=junk[:, 512:1024].rearrange("p (h d) -> p h d", d=128), in0=osum[:, tl, :].rearrange("p (h d) -> p h d", d=128),
                                                                         in1=rs4[:].unsqueeze(2).to_broadcast([128, 4, 128]), op=ALU.mult), R=["rs4"] + [("osum", tl, h_) for h_ in range(4)], W=["tmp"])
                            S.op("pool", lambda e, tl=tl, lt=lt: e.tensor_mul(OF[:, lt, :], junk[:, 512:1024], sg[cb][:, tl, :]), R=["tmp", ("sg", cb, tl)], W=[("OF", lt, h_) for h_ in range(4)])
                    S.flush(pend)
                prologue(chunks[0], 0)
                for idx, ci in enumerate(chunks):
                    cb = idx % 2
                    pend = []
                    if idx + 1 < len(chunks):
                        S.defer = pend
                        prologue(chunks[idx + 1], 1 - cb)
                        S.defer = None
                    hgrn(ci, cb, pend)
                if dbg and stage == 2 and dirn == 1:
                    S.barrier()
                    S.dma("sp", lambda e: e.dma_start(out=DO("d_osum", [128, 1024], F32)[:, :], in_=osum[:].rearrange("p a b -> p (a b)")), "dd_osum")
                    S.dma("sp", lambda e: e.dma_start(out=DO("d_sg", [128, 1024], BF16)[:, :], in_=sg[0][:].rearrange("p a b -> p (a b)")), "dd_sg")
                    for nm, t, n, dt in (("hbb", hbb, 256, F32), ("hc", hc, 256, F32), ("hk", hk, 256, F32), ("Eend", Eend[1], 4, F32)):
                        S.dma("sp", lambda e, nm=nm, t=t, n=n, dt=dt: e.dma_start(out=DO("d_" + nm, [128, n], dt)[:, :], in_=t[:]), "dd_" + nm)
                    S.dma("sp", lambda e: e.dma_start(out=DO("d_S32", [128, 512], F32)[:, :], in_=S32[:].rearrange("p a b -> p (a b)")), "dd_S32")
                    S.dma("sp", lambda e: e.dma_start(out=DO("d_QEA", [128, 256], BF16)[:, :], in_=QEA[0][:].rearrange("p a b -> p (a b)")), "dd_QEA")
                    S.dma("sp", lambda e: e.dma_start(out=DO("d_KDB", [128, 256], BF16)[:, :], in_=KDB[0][:].rearrange("p a b -> p (a b)")), "dd_KDB")
            S.barrier()

        if stage >= 1:
            pass_fb(0)
        if dbg and stage == 1:
            S.dma("sp", lambda e: e.dma_start(out=DO("d_QT", [128, 4 * SEQ], BF16)[:, :], in_=QO[:].rearrange("p a b t -> p (a b t)")), "o0", R=[])
            S.dma("sp", lambda e: e.dma_start(out=DO("d_KT", [128, 2 * TT], BF16)[:, :], in_=KTd[:].rearrange("p a t -> p (a t)")), "o1", R=[])
            S.dma("sp", lambda e: e.dma_start(out=DO("d_V", [128, NT * 132], BF16)[:, :], in_=Vaug[:].rearrange("p a b c -> p (a b c)")), "o2", R=[])
            S.dma("sp", lambda e: e.dma_start(out=DO("d_OF", [128, NLT * 512], BF16)[:, :], in_=OF[:].rearrange("p a t -> p (a t)")), "o3", R=[])
            S.barrier()
        if stage >= 2:
            pass_fb(1)
            if dbg and stage == 2:
                S.dma("sp", lambda e: e.dma_start(out=DO("d_OF", [128, NLT * 512], BF16)[:, :], in_=OF[:].rearrange("p a t -> p (a t)")), "o3", R=[])
                S.barrier()


        def attention():
            with ExitStack() as st:
                PT = [[SB(st, "PT%d_%d" % (a_, i), [128, 512], BF16) for i in range(4)] for a_ in range(2)]
                OT = [SB(st, "OT%d" % i, [65, 512], F32) for i in range(2)]
                OA = SB(st, "OA", [128, 4, 8, 65], F32)
                rl = SB(st, "rl", [128, 8], F32)
                ss8 = SB(st, "ss8", [128, 8], F32)
                sc8 = SB(st, "sc8", [128, 8], F32)
                ajunk = SB(st, "ajunk", [128, 8, 64], F32)
                pS = [[PS(st, "pS%d_%d" % (a_, i), [128, 512], F32) for i in range(2)] for a_ in range(2)]
                pO = [PS(st, "pO%d" % i, [128, 512], F32) for i in range(2)]
                pOT = [PS(st, "pOT%d" % i, [128, 512], F32) for i in range(2)]
                for qb in range(8 if (LIM[0] > 8 or stage != 3) else LIM[0]):
                    for pair in range(4):
                        kv = pair // 2

                        def qk(g):
                            for a_ in range(2):
                                hp = a_ * 64
                                S.op("pe", lambda e: e.matmul(pS[a_][g % 2][:], lhsT=KTd[hp:hp + 64, kv, g * 128:(g + 1) * 128],
                                                              rhs=QO[hp:hp + 64, qb, pair, :], start=True, stop=True),
                                     R=[("QO", qb)], W=[("pS", a_, g % 2)], signal=(a_ == 1))
                        qk(0)
                        for g in range(NT):
                            if g + 1 < NT:
                                qk(g + 1)
                            for a_ in range(2):
                                S.op("act", lambda e: e.activation(out=PT[a_][g % 4][:], in_=pS[a_][g % 2][:], func=AF.Exp, scale=0.125),
                                     W=[("pS", a_, g % 2), ("PT", a_, g % 4)])
                            for a_ in range(2):
                                S.op("pe", lambda e: e.matmul(pO[a_][0:65, :], lhsT=Vaug[:, g, kv, 0:65], rhs=PT[a_][g % 4][:], start=(g == 0), stop=(g == NT - 1)),
                                     R=[("PT", a_, g % 4)], W=[("pO", a_)], signal=(g == NT - 1 or a_ == 1))
                        for a_ in range(2):
                            h = pair * 2 + a_
                            S.op("dve", lambda e: e.tensor_copy(OT[a_][:], pO[a_][0:65, :]), W=[("pO", a_), ("OT", a_)])
                            for j in range(4):
                                S.op("pe", lambda e: e.transpose(pOT[a_][:, j * 65:(j + 1) * 65], OT[a_][0:65, j * 128:(j + 1) * 128], identf[0:65, 0:65]),
                                     R=[("OT", a_), "cst"], W=[("pOT", a_)], signal=(j == 3))
                            S.op("dve", lambda e: e.tensor_copy(OA[:, :, h, :], pOT[a_][:, 0:260].rearrange("p (j c) -> p j c", j=4)), W=[("pOT", a_), ("OA", h)])
                    for j in range(4):
                        oaj = OA[:, j, :, 0:64]
                        S.op("dve", lambda e: e.reciprocal(rl[:], OA[:, j, :, 64]), R=[("OA", h_) for h_ in range(8)], W=["rl"])
                        S.op("dve", lambda e: e.tensor_tensor(out=ajunk[:], in0=oaj, in1=oaj, op=ALU.mult), R=[("OA", h_) for h_ in range(8)], W=["ajunk"])
                        S.op("dve", lambda e: e.tensor_reduce(out=ss8[:], in_=ajunk[:], axis=AX.X, op=ALU.add), R=["ajunk"], W=["ss8"])
                        S.op("dve", lambda e: e.tensor_mul(ss8[:], ss8[:], rl[:]), R=["ss8", "rl"], W=["ss8"])
                        S.op("dve", lambda e: e.tensor_mul(ss8[:], ss8[:], rl[:]), R=["ss8", "rl"], W=["ss8"])
                        S.op("act", lambda e: e.activation(out=sc8[:], in_=ss8[:], func=AF.Ln, scale=1.0 / 64, bias=epst[:]), R=["ss8", "eps"], W=["sc8"])
                        S.op("act", lambda e: e.activation(out=sc8[:], in_=sc8[:], func=AF.Exp, scale=-0.5), R=["sc8"], W=["sc8"])
                        S.op("dve", lambda e: e.tensor_mul(sc8[:], sc8[:], rl[:]), R=["sc8", "rl"], W=["sc8"])
                        S.op("dve", lambda e: e.tensor_tensor(out=QO[:, qb, j, :].rearrange("p (h d) -> p h d", d=64), in0=oaj,
                                                              in1=sc8[:].unsqueeze(2).to_broadcast([128, 8, 64]), op=ALU.mult),
                             R=["sc8"] + [("OA", h_) for h_ in range(8)], W=[("QO", qb)])
            S.barrier()

        if stage >= 3:
            attention()
            if dbg and stage == 3:
                S.dma("sp", lambda e: e.dma_start(out=DO("d_OAN", [128, 4 * SEQ], BF16)[:, :], in_=QO[:].rearrange("p a b t -> p (a b t)")), "o0", R=[])
                S.barrier()


        kvst.close()
        G1, B2, A2, G2 = A1, B1, cA1, cB1
        M1 = SB(top, "M1", [128, NLT, 32], F32)
        M2 = SB(top, "M2", [128, NLT, 32], F32)
        Msum = SB(top, "Msum", [128, NLT, 32], BF16)
        W12 = SB(top, "W12", [128, NLT, 2], F32)
        SLOT = SB(top, "SLOT", [128, NBLK, 2], I32)
        IDXW = SB(top, "IDXW", [128, NBLK], I32)
        IDXS = SB(top, "IDXS", [128, 2, NBLK], I32)

        def phase_d():
            with ExitStack() as st0:
                sc2 = SB(st0, "sc2", [128, DM], F32)
                g2 = SB(st0, "g2", [128, DM], F32)
                outs = {4: (G1[:, 0:512], None), 5: (G1[:, 512:1024], None), 6: (B2[:, 0:512], None), 7: (B2[:, 512:1024], None),
                        8: (sc2[:, 0:512], None), 9: (sc2[:, 512:1024], None), 10: (G2[:, 0:512], None), 11: (G2[:, 512:1024], None)}
                adaln_phase(list(range(4, 12)), outs, None)
                S.dma("sp", lambda e: e.dma_start(out=g2[:], in_=n2g_d[0:1, :].partition_broadcast(128)), "g2", W=["g2"])
                S.op("dve", lambda e: e.scalar_tensor_tensor(out=A2[:], in0=sc2[:], scalar=1.0, in1=g2[:], op0=ALU.add, op1=ALU.mult), R=["g2"], W=["A2"])
                S.barrier()
            with ExitStack() as st:
                Wout = SB(st, "Wout", [128, 8, DM], BF16)
                stg = [SB(st, "stg%d" % i, [128, DM], F32) for i in range(2)]
                aog = SB(st, "aog", [128, 8], F32)
                Wr = SB(st, "Wr", [128, 8, 36], BF16)
                brb = SB(st, "brb", [128, 36], F32)
                catT = [SB(st, "catT%d" % i, [128, 8, 128], BF16) for i in range(2)]
                xt = [SB(st, "dxt%d" % i, [128, DM], F32) for i in range(2)]
                x1 = [SB(st, "x1%d" % i, [128, DM], F32) for i in range(2)]
                tmp = SB(st, "dtmp", [128, DM], F32)
                h2b = [SB(st, "h2b%d" % i, [128, DM], BF16) for i in range(2)]
                h2T = SB(st, "h2T", [128, 8, 128], BF16)
                zt = SB(st, "zt", [128, DM], BF16)
                ss = SB(st, "dss", [128, 1], F32)
                rs = SB(st, "drs", [128, 1], F32)
                LG = SB(st, "LG", [128, NLT, 36], F32)
                gmx = SB(st, "gmx", [128, NLT], F32)
                pgv = SB(st, "pgv", [128, NLT], F32)
                ohg = SB(st, "ohg", [128, NLT, 4], F32)
                eg = SB(st, "eg", [128, NLT, 4], F32)
                prod = SB(st, "prod", [128, NLT, 8, 4], F32)
                les = SB(st, "les", [128, NLT, 8], F32)
                les2 = SB(st, "les2", [128, NLT, 8], F32)
                oh1 = SB(st, "oh1", [128, NLT, 8], F32)
                oh2 = SB(st, "oh2", [128, NLT, 8], F32)
                m1 = SB(st, "m1", [128, NLT], F32)
                m2 = SB(st, "m2", [128, NLT], F32)
                ptr = [PS(st, "dptr%d" % i, [128, DM], BF16) for i in range(2)]
                ptr2 = PS(st, "dptr2", [128, DM], BF16)
                pmix = [PS(st, "pmix%d" % i, [128, DM], F32) for i in range(2)]
                pr = PS(st, "pr", [128, 512], F32)
                S.dma("sp", lambda e: e.dma_start(out=aog[:], in_=aog_d[:, :]), "aog", W=["aog"])
                S.dma("pool", lambda e: e.dma_start(out=Wr[:], in_=wr_d[:, :].rearrange("(c p) n -> p c n", p=128)), "Wr", W=["Wr"])
                S.dma("sp", lambda e: e.dma_start(out=brb[:], in_=br_d[0:1, :].partition_broadcast(128)), "brb", W=["brb"])
                S.op("dve", lambda e: e.memset(zt[:], 0.0), W=["zt"])
                S.dma("sp", lambda e: e.dma_start(out=h2s_d[SEQ:OUT_ROWS, :], in_=zt[:]), "zt", R=["zt"])
                for k in range(8):
                    S.dma("sp", lambda e: e.dma_start(out=stg[k % 2][:], in_=wout_d[k * 128:(k + 1) * 128, :]), "stg%d" % (k % 2), W=[("stg", k % 2)])
                    S.op("dve", lambda e: e.tensor_scalar(out=Wout[:, k, :], in0=stg[k % 2][:], scalar1=aog[:, k:k + 1], scalar2=None, op0=ALU.mult),
                         R=[("stg", k % 2), "aog"], W=["Wout"])
                ntl = NLT if (LIM[0] > NLT or stage != 4) else LIM[0]

                def stage_a(lt):
                    b2 = lt % 2
                    for k in range(8):
                        src = QO[:, lt // 4, lt % 4, k * 128:(k + 1) * 128] if k < 4 else OF[:, lt, (k - 4) * 128:(k - 3) * 128]
                        S.op("pe", lambda e: e.transpose(ptr[b2][:, k * 128:(k + 1) * 128], src, ident[:]), R=["ident"], W=[("dptr", b2)], signal=(k == 7))
                    S.op("act", lambda e: e.activation(out=catT[b2][:], in_=ptr[b2][:].rearrange("p (k t) -> p k t", k=8), func=AF.Copy), W=[("dptr", b2), ("catT", b2)])
                    for nh in range(2):
                        for k in range(8):
                            S.op("pe", lambda e: e.matmul(pmix[b2][:, nh * 512:(nh + 1) * 512], lhsT=catT[b2][:, k, :], rhs=Wout[:, k, nh * 512:(nh + 1) * 512], start=(k == 0), stop=(k == 7)),
                                 R=[("catT", b2), "Wout"], W=[("pmix", b2)], signal=(k == 7 and nh == 1))
                    S.dma("sp", lambda e: e.dma_start(out=xt[b2][:], in_=x_d[lt * 128:(lt + 1) * 128, :]), "dxt%d" % b2, W=[("dxt", b2)])
                stage_a(0)
                for lt in range(ntl):
                    b2 = lt % 2
                    S.op("dve", lambda e: e.tensor_tensor(out=tmp[:], in0=pmix[b2][:], in1=G1[:], op=ALU.mult), W=[("pmix", b2), "dtmp"])
                    S.op("pool", lambda e: e.tensor_add(x1[b2][:], tmp[:], xt[b2][:]), R=["dtmp", ("dxt", b2)], W=[("x1", b2)])
                    if lt + 1 < ntl:
                        stage_a(lt + 1)
                    S.dma("sp", lambda e: e.dma_start(out=out_d[lt * 128:(lt + 1) * 128, :], in_=x1[b2][:]), "x1o%d" % b2, R=[("x1", b2)])
                    S.op("act", lambda e: e.activation(out=tmp[:], in_=x1[b2][:], func=AF.Square, accum_out=ss[:]), R=[("x1", b2)], W=["dtmp", "dss"])
                    S.op("act", lambda e: e.activation(out=rs[:], in_=ss[:], func=AF.Sqrt, scale=1.0 / DM, bias=epst[:]), R=["dss", "eps"], W=["drs"])
                    S.op("dve", lambda e: e.reciprocal(rs[:], rs[:]), R=["drs"], W=["drs"])
                    S.op("dve", lambda e: e.scalar_tensor_tensor(out=tmp[:], in0=x1[b2][:], scalar=rs[:, 0:1], in1=A2[:], op0=ALU.mult, op1=ALU.mult),
                         R=[("x1", b2), "drs"], W=["dtmp"])
                    S.op("pool", lambda e: e.tensor_add(h2b[b2][:], tmp[:], B2[:]), R=["dtmp"], W=[("h2b", b2)])
                    S.dma("sp", lambda e: e.dma_start(out=h2s_d[lt * 128:(lt + 1) * 128, :], in_=h2b[b2][:]), "h2o%d" % b2, R=[("h2b", b2)])
                    for k in range(8):
                        S.op("pe", lambda e: e.transpose(ptr2[:, k * 128:(k + 1) * 128], h2b[b2][:, k * 128:(k + 1) * 128], ident[:]), R=[("h2b", b2), "ident"], W=["dptr2"], signal=(k == 7))
                    S.op("act", lambda e: e.activation(out=h2T[:], in_=ptr2[:].rearrange("p (k t) -> p k t", k=8), func=AF.Copy), W=["dptr2", "h2T"])
                    for k in range(8):
                        S.op("pe", lambda e: e.matmul(pr[:, 0:36], lhsT=h2T[:, k, :], rhs=Wr[:, k, :], start=(k == 0), stop=(k == 7)), R=["h2T", "Wr"], W=["pr"], signal=(k == 7))
                    S.op("dve", lambda e: e.tensor_add(LG[:, lt, :], pr[:, 0:36], brb[:]), R=["brb"], W=["pr", ("LG", lt)])
                T = NLT
                lgG = LG[:, :, 0:4]
                allLG = [("LG", t_) for t_ in range(NLT)]
                S.op("dve", lambda e: e.tensor_reduce(out=gmx[:], in_=lgG, axis=AX.X, op=ALU.max), R=allLG, W=["gmx"])
                S.op("dve", lambda e: e.tensor_tensor(out=ohg[:], in0=lgG, in1=gmx[:].unsqueeze(2).to_broadcast([128, T, 4]), op=ALU.is_equal), R=allLG + ["gmx"], W=["ohg"])
                S.op("dve", lambda e: e.tensor_tensor(out=eg[:], in0=lgG, in1=gmx[:].unsqueeze(2).to_broadcast([128, T, 4]), op=ALU.subtract), R=allLG + ["gmx"], W=["eg"])
                S.op("act", lambda e: e.activation(out=eg[:], in_=eg[:], func=AF.Exp), R=["eg"], W=["eg"])
                S.op("dve", lambda e: e.tensor_reduce(out=pgv[:], in_=eg[:], axis=AX.X, op=ALU.add), R=["eg"], W=["pgv"])
                S.op("dve", lambda e: e.reciprocal(pgv[:], pgv[:]), R=["pgv"], W=["pgv"])
                lex = LG[:, :, 4:36].rearrange("p t (g x) -> p t x g", g=4)
                S.op("dve", lambda e: e.tensor_tensor(out=prod[:], in0=lex, in1=ohg[:].unsqueeze(2).to_broadcast([128, T, 8, 4]), op=ALU.mult), R=allLG + ["ohg"], W=["prod"])
                S.op("dve", lambda e: e.tensor_reduce(out=les[:], in_=prod[:], axis=AX.X, op=ALU.add), R=["prod"], W=["les"])
                S.op("dve", lambda e: e.tensor_reduce(out=m1[:], in_=les[:], axis=AX.X, op=ALU.max), R=["les"], W=["m1"])
                S.op("dve", lambda e: e.tensor_tensor(out=oh1[:], in0=les[:], in1=m1[:].unsqueeze(2).to_broadcast([128, T, 8]), op=ALU.is_equal), R=["les", "m1"], W=["oh1"])
                S.op("dve", lambda e: e.scalar_tensor_tensor(out=les2[:], in0=oh1[:], scalar=-1e30, in1=les[:], op0=ALU.mult, op1=ALU.add), R=["oh1", "les"], W=["les2"])
                S.op("dve", lambda e: e.tensor_reduce(out=m2[:], in_=les2[:], axis=AX.X, op=ALU.max), R=["les2"], W=["m2"])
                S.op("dve", lambda e: e.tensor_tensor(out=oh2[:], in0=les2[:], in1=m2[:].unsqueeze(2).to_broadcast([128, T, 8]), op=ALU.is_equal), R=["les2", "m2"], W=["oh2"])
                S.op("dve", lambda e: e.tensor_sub(m1[:], m1[:], m2[:]), R=["m1", "m2"], W=["m1"])
                S.op("act", lambda e: e.activation(out=m2[:], in_=m1[:], func=AF.Sigmoid), R=["m1"], W=["m2"])
                S.op("dve", lambda e: e.tensor_mul(W12[:, :, 0], m2[:], pgv[:]), R=["m2", "pgv"], W=["W12a"])
                S.op("dve", lambda e: e.tensor_sub(W12[:, :, 1], pgv[:], W12[:, :, 0]), R=["pgv", "W12a"], W=["W12b"])
                S.op("dve", lambda e: e.tensor_tensor(out=M1[:].rearrange("p t (g x) -> p t g x", g=4), in0=ohg[:].unsqueeze(3).to_broadcast([128, T, 4, 8]),
                                                      in1=oh1[:].unsqueeze(2).to_broadcast([128, T, 4, 8]), op=ALU.mult), R=["ohg", "oh1"], W=["M1"])
                S.op("dve", lambda e: e.tensor_tensor(out=M2[:].rearrange("p t (g x) -> p t g x", g=4), in0=ohg[:].unsqueeze(3).to_broadcast([128, T, 4, 8]),
                                                      in1=oh2[:].unsqueeze(2).to_broadcast([128, T, 4, 8]), op=ALU.mult), R=["ohg", "oh2"], W=["M2"])
                S.op("dve", lambda e: e.tensor_add(Msum[:].rearrange("p t x -> p (t x)"), M1[:].rearrange("p t x -> p (t x)"), M2[:].rearrange("p t x -> p (t x)")), R=["M1", "M2"], W=["Msum"])
            S.barrier()

        if stage >= 4:
            phase_d()
            if dbg and stage == 4:
                S.dma("sp", lambda e: e.dma_start(out=DO("d_M1", [128, NLT * 32])[:, :], in_=M1[:].rearrange("p a b -> p (a b)")), "o0")
                S.dma("sp", lambda e: e.dma_start(out=DO("d_M2", [128, NLT * 32])[:, :], in_=M2[:].rearrange("p a b -> p (a b)")), "o1")
                S.dma("sp", lambda e: e.dma_start(out=DO("d_W12", [128, NLT * 2])[:, :], in_=W12[:].rearrange("p a b -> p (a b)")), "o2")
                S.barrier()


        def routing():
            with ExitStack() as st:
                cnt = SB(st, "cnt", [128, 32], F32)
                cmp3 = SB(st, "cmp3", [128, NBLK, 32], F32)
                nblk = SB(st, "nblk", [128, 32], F32)
                padded = SB(st, "padded", [128, 32], F32)
                pend = SB(st, "pend", [128, 32], F32)
                pstart = SB(st, "pstart", [128, 32], F32)
                ones32 = SB(st, "ones32", [128, 32], F32)
                blke = SB(st, "blke", [128, NBLK], F32)
                same = SB(st, "same", [128, NBLK], F32)
                slotx = SB(st, "slotx", [128, NBLK], F32)
                tokf = SB(st, "tokf", [128, 32], F32)
                slotf = SB(st, "slotf", [128, 32], F32)
                t1 = SB(st, "t1", [128, 32], F32)
                DEST = SB(st, "DEST", [128, NLT, 2], F32)
                DESTI = SB(st, "DESTI", [128, NLT, 2], I32)
                REC = SB(st, "REC", [128, NLT, 2, 2], F32)
                PRE = SB(st, "PRE", [128, NBLK, 2], F32)
                pref = SB(st, "pref", [128, 1], F32)
                pcnt = PS(st, "pcnt", [128, 512], F32)
                ppos = PS(st, "ppos", [128, 512], F32)
                RECi = REC[:].bitcast(I32)
                PREi = PRE[:].bitcast(I32)
                S.op("dve", lambda e: e.memset(PRE[:].rearrange("p a b -> p (a b)"), 0.0), W=["PRE"])
                S.op("dve", lambda e: e.tensor_scalar(out=pref[:], in0=iota_p, scalar1=float(SEQ), scalar2=None, op0=ALU.add), R=["cst"], W=["pref"])
                S.op("dve", lambda e: e.tensor_copy(PREi[:, :, 0], pref[:].to_broadcast([128, NBLK])), R=["pref", "PRE"], W=["PRE"])
                S.dma("sp", lambda e: e.dma_start(out=slots_d.rearrange("(j p) r -> p j r", p=128), in_=PREi), "pre", R=["PRE"], W=["slots"])
                S.op("dve", lambda e: e.memset(ones32[:], 1.0), W=["ones32"])
                for lt in range(NLT):
                    S.op("pe", lambda e: e.matmul(pcnt[:, 0:32], lhsT=ones_bf[:], rhs=Msum[:, lt, :], start=(lt == 0), stop=(lt == NLT - 1)),
                         R=["ones"], W=["pcnt"], signal=(lt == NLT - 1))
                S.op("dve", lambda e: e.tensor_copy(cnt[:], pcnt[:, 0:32]), W=["pcnt", "cnt"])
                S.op("dve", lambda e: e.tensor_tensor(out=cmp3[:, 0:32, :], in0=cnt[:].unsqueeze(2).to_broadcast([128, 32, 32]),
                                                      in1=blk128[:, 0:32].unsqueeze(1).to_broadcast([128, 32, 32]), op=ALU.is_gt), R=["cnt", "cst"], W=["cmp3"])
                S.op("dve", lambda e: e.tensor_reduce(out=nblk[:], in_=cmp3[:, 0:32, :], axis=AX.X, op=ALU.add), R=["cmp3"], W=["nblk"])
                S.op("dve", lambda e: e.tensor_scalar(out=padded[:], in0=nblk[:], scalar1=128.0, scalar2=None, op0=ALU.mult), R=["nblk"], W=["padded"])
                S.op("dve", lambda e: e.tensor_tensor_scan(out=pend[:], data0=ones32[:], data1=padded[:], initial=0.0, op0=ALU.mult, op1=ALU.add), R=["ones32", "padded"], W=["pend"])
                S.op("dve", lambda e: e.tensor_sub(pstart[:], pend[:], padded[:]), R=["pend", "padded"], W=["pstart"])
                S.op("dve", lambda e: e.tensor_tensor(out=cmp3[:], in0=blk128[:, 0:NBLK].unsqueeze(2).to_broadcast([128, NBLK, 32]),
                                                      in1=pend[:].unsqueeze(1).to_broadcast([128, NBLK, 32]), op=ALU.is_ge), R=["pend", "cst", "nblk"], W=["cmp3"])
                S.op("dve", lambda e: e.tensor_reduce(out=blke[:], in_=cmp3[:], axis=AX.X, op=ALU.add), R=["cmp3"], W=["blke"])
                S.op("dve", lambda e: e.tensor_scalar(out=blke[:], in0=blke[:], scalar1=31.0, scalar2=None, op0=ALU.min), R=["blke"], W=["blke"])
                S.op("dve", lambda e: e.memset(same[:], 0.0), W=["same"])
                S.op("dve", lambda e: e.tensor_tensor(out=same[:, 1:NBLK], in0=blke[:, 1:NBLK], in1=blke[:, 0:NBLK - 1], op=ALU.is_equal), R=["blke", "same"], W=["same"])
                S.op("dve", lambda e: e.tensor_scalar(out=blke[:], in0=blke[:], scalar1=128.0, scalar2=None, op0=ALU.mult), R=["blke", "same"], W=["blke"])
                S.op("dve", lambda e: e.tensor_scalar(out=blke[:], in0=blke[:], scalar1=iota_p, scalar2=None, op0=ALU.add), R=["blke", "cst"], W=["blke"])
                S.op("dve", lambda e: e.tensor_copy(IDXW[:], blke[:]), R=["blke"], W=["IDXW"])
                S.op("dve", lambda e: e.scalar_tensor_tensor(out=same[:], in0=same[:], scalar=8192.0, in1=blke[:], op0=ALU.mult, op1=ALU.add), R=["blke", "same"], W=["same"])
                for hh in range(2):
                    S.op("dve", lambda e: e.tensor_scalar(out=slotx[:], in0=same[:], scalar1=2.0, scalar2=float(hh), op0=ALU.mult, op1=ALU.add), R=["same"], W=["slotx"])
                    S.op("dve", lambda e: e.tensor_copy(IDXS[:, hh, :], slotx[:]), R=["slotx"], W=["IDXS"])
                S.op("dve", lambda e: e.tensor_scalar(out=tokf[:], in0=blk128[:, 0:32], scalar1=iota_p, scalar2=None, op0=ALU.add), R=["cst"], W=["tokf"])
                for k in range(2):
                    S.op("dve", lambda e: e.tensor_copy(RECi[:, :, k, 0], tokf[:]), R=["tokf"], W=[("REC", k, 0)])
                    S.op("dve", lambda e: e.tensor_copy(REC[:, :, k, 1], W12[:, :, k]), W=[("REC", k, 1)])
                for lt in range(NLT):
                    for j in range(lt):
                        S.op("pe", lambda e: e.matmul(ppos[:, 0:32], lhsT=ones_bf[:], rhs=Msum[:, j, :], start=(j == 0), stop=False), R=["ones"], W=["ppos"], signal=False)
                    S.op("pe", lambda e: e.matmul(ppos[:, 0:32], lhsT=ustr[:], rhs=Msum[:, lt, :], start=(lt == 0), stop=True), R=["ustr"], W=["ppos"])
                    S.op("dve", lambda e: e.tensor_add(slotf[:], ppos[:, 0:32], pstart[:]), R=["pstart"], W=["ppos", "slotf"])
                    for k, Mk in enumerate((M1, M2)):
                        S.op("dve", lambda e: e.tensor_mul(t1[:], slotf[:], Mk[:, lt, :]), R=["slotf"], W=["t1"])
                        S.op("dve", lambda e: e.tensor_reduce(out=DEST[:, lt, k:k + 1], in_=t1[:], axis=AX.X, op=ALU.add), R=["t1"], W=[("DEST", lt, k)])
                S.op("dve", lambda e: e.tensor_copy(DESTI[:].rearrange("p a b -> p (a b)"), DEST[:].rearrange("p a b -> p (a b)")),
                     R=[("DEST", lt, k) for lt in range(NLT) for k in range(2)], W=["DESTI"])
                S.barrier()
                n = 0
                for lt in range(NLT):
                    for k in range(2):
                        S.dma("pool", lambda e: e.indirect_dma_start(out=slots_d[:, :], out_offset=bass.IndirectOffsetOnAxis(ap=DESTI[:, lt, k:k + 1], axis=0),
                                                                     in_=RECi[:, lt, k, :], in_offset=None), "sc%d" % (n % 4))
                        n += 1
                S.barrier()
                S.dma("sp", lambda e: e.dma_start(out=SLOT[:], in_=slots_d.rearrange("(j p) r -> p j r", p=128)), "slotrd", W=["SLOT"])
                S.barrier()

        def phase_e():
            with ExitStack() as st:
                Wg = [SB(st, "Wg0", [128, 8, 512], BF16)]
                Wu = [SB(st, "Wu0", [128, 8, 512], BF16)]
                Wd = [SB(st, "Wd0", [128, 4, DM], BF16)]
                xb = [SB(st, "xb%d" % i, [128, DM], BF16) for i in range(2)]
                xbT = SB(st, "xbT", [128, 8, 128], BF16)
                sil = SB(st, "sil", [128, 512], F32)
                atm = SB(st, "atm", [128, 512], BF16)
                aT = SB(st, "aT", [128, 4, 128], BF16)
                ys = [SB(st, "ys%d" % i, [128, DM], F32) for i in range(2)]
                ptr = PS(st, "eptr", [128, DM], BF16)
                pg = PS(st, "epg", [128, 512], F32)
                pu = PS(st, "epu", [128, 512], F32)
                paT = PS(st, "epaT", [128, DM], BF16)
                py = PS(st, "epy", [128, DM], F32)
                SLOTF = SLOT[:].bitcast(F32)
                nb = NBLK if (LIM[0] > NBLK or stage != 5) else LIM[0]

                bcreg = nc.gpsimd.alloc_register("bcreg")
                nc.gpsimd.reg_mov(bcreg, 8191)

                def gather(j):
                    b = j % 2
                    S.dma("pool", lambda e: e.indirect_dma_start(out=xb[b][:], out_offset=None, in_=h2s_d[:, :], in_offset=bass.IndirectOffsetOnAxis(ap=SLOT[:, j, 0:1], axis=0)),
                          "xb%d" % b, R=["SLOT"], W=[("xb", b)])

                def convert(j=None, which=(0, 1, 2)):
                    if j is None:
                        return
                    for nm, W_, src in [(("Wg", Wg[0], weg_d), ("Wu", Wu[0], weu_d), ("Wd", Wd[0], wed_d))[w_] for w_ in which]:
                        src2 = src.rearrange("r c n -> r (c n)").rearrange("r (h m) -> (r h) m", h=2)
                        dst2 = W_[:].rearrange("p c n -> p (c n)")
                        for hh in range(2):
                            ix = IDXS[:, hh, j:j + 1]
                            S.dma("pool", lambda e: e.indirect_dma_start(out=dst2[:, hh * 2048:(hh + 1) * 2048], out_offset=None, in_=src2,
                                                                         in_offset=bass.IndirectOffsetOnAxis(ap=ix, axis=0), bounds_check=bcreg, oob_is_err=False),
                                  "%s%d" % (nm, hh), R=["IDXS"], W=[(nm, hh)])
                gather(0)
                convert(0)
                for j in range(nb):
                    b = j % 2
                    if j + 1 < nb:
                        gather(j + 1)
                    for k in range(8):
                        S.op("pe", lambda e: e.transpose(ptr[:, k * 128:(k + 1) * 128], xb[b][:, k * 128:(k + 1) * 128], ident[:]), R=[("xb", b), "ident"], W=["eptr"], signal=(k == 7))
                    S.op("act", lambda e: e.activation(out=xbT[:], in_=ptr[:].rearrange("p (k t) -> p k t", k=8), func=AF.Copy), W=["eptr", "xbT"])
                    for k in range(8):
                        S.op("pe", lambda e: e.matmul(pg[:], lhsT=xbT[:, k, :], rhs=Wg[0][:, k, :], start=(k == 0), stop=(k == 7)), R=["xbT", ("Wg", 0), ("Wg", 1)], W=["epg"], signal=False)
                        S.op("pe", lambda e: e.matmul(pu[:], lhsT=xbT[:, k, :], rhs=Wu[0][:, k, :], start=(k == 0), stop=(k == 7)), R=["xbT", ("Wu", 0), ("Wu", 1)], W=["epu"], signal=(k == 7))
                    if j + 1 < nb:
                        convert(j + 1, which=(0, 1))
                    S.op("act", lambda e: e.activation(out=sil[:], in_=pg[:], func=AF.Silu), W=["epg", "sil"])
                    S.op("dve", lambda e: e.tensor_tensor(out=atm[:], in0=pu[:], in1=sil[:], op=ALU.mult), R=["sil"], W=["epu", "atm"])
                    for f in range(4):
                        S.op("pe", lambda e: e.transpose(paT[:, f * 128:(f + 1) * 128], atm[:, f * 128:(f + 1) * 128], ident[:]), R=["atm", "ident"], W=["epaT"], signal=(f == 3))
                    S.op("act", lambda e: e.activation(out=aT[:], in_=paT[:, 0:512].rearrange("p (f t) -> p f t", f=4), func=AF.Copy), W=["epaT", "aT"])
                    for nh in range(2):
                        for f in range(4):
                            S.op("pe", lambda e: e.matmul(py[:, nh * 512:(nh + 1) * 512], lhsT=aT[:, f, :], rhs=Wd[0][:, f, nh * 512:(nh + 1) * 512], start=(f == 0), stop=(f == 3)),
                                 R=["aT", ("Wd", 0), ("Wd", 1)], W=["epy"], signal=(f == 3 and nh == 1))
                    S.op("dve", lambda e: e.scalar_tensor_tensor(out=ys[b][:], in0=py[:], scalar=SLOTF[:, j, 1:2], in1=G2[:], op0=ALU.mult, op1=ALU.mult),
                         R=["SLOT"], W=["epy", ("ys", b)])
                    if j + 1 < nb:
                        convert(j + 1, which=(2,))
                    if LIM[1] & 8:
                        S.dma("sp", lambda e: e.dma_start(out=DO("d_ys%d" % j, [128, DM])[:, :], in_=ys[b][:]), "dys%d" % b, R=[("ys", b)])
                        continue
                    S.dma("pool", lambda e: e.indirect_dma_start(out=out_d[:, :], out_offset=bass.IndirectOffsetOnAxis(ap=SLOT[:, j, 0:1], axis=0),
                                                                 in_=ys[b][:], in_offset=None, compute_op=ALU.add), "scat", R=[("ys", b), "SLOT"], W=["outd"])
            S.barrier()

        if stage >= 5:
            routing()
            if dbg and stage == 5:
                S.dma("sp", lambda e: e.dma_start(out=DO("d_SLOT", [128, NBLK * 2], I32)[:, :], in_=SLOT[:].rearrange("p a b -> p (a b)")), "o0")
                S.dma("sp", lambda e: e.dma_start(out=DO("d_IDXW", [128, NBLK], I32)[:, :], in_=IDXW[:]), "o1")
                S.barrier()
            if not (LIM[1] & 4):
                phase_e()

        if dbg and stage == 0:
            S.dma("sp", lambda e: e.dma_start(out=DO("d_A1", [128, DM])[:, :], in_=A1[:]), "o0", R=["A1"])
            S.dma("sp", lambda e: e.dma_start(out=DO("d_B1", [128, DM])[:, :], in_=B1[:]), "o1", R=["B1"])
            S.dma("sp", lambda e: e.dma_start(out=DO("d_cA1", [128, DM])[:, :], in_=cA1[:]), "o2", R=["cA1"])
            S.dma("sp", lambda e: e.dma_start(out=DO("d_lb", [128, 8])[:, :], in_=lbv[:]), "o3", R=["lbv"])
        S.barrier()
    return nc, dbg_d


def host_consts():
    c = np.zeros((128, 1152), np.float32)
    c[:, 0:128] = np.eye(128)
    s = np.arange(128)[:, None]
    t = np.arange(128)[None, :]
    c[:, 128:256] = (s <= t)
    c[:, 256:384] = (s >= t)
    seg = np.ones(256, np.float32)
    seg[0::64] = 0
    c[:, 384:640] = seg[None, :]
    c[:, 640] = np.arange(128)
    same = (s // 64) == (t // 64)
    c[:, 896:1024] = (s <= t) & same
    c[:, 1024:1152] = (s >= t) & same
    c[:, 656:752] = (np.arange(96) * 128)[None, :]
    half = 32
    freqs = (10000.0 ** (-np.arange(0, half, 2, dtype=np.float32) / half)).astype(np.float32)
    tok = np.arange(SEQ)
    row = (tok // 64).astype(np.float32)
    col = (tok % 64).astype(np.float32)
    ang = np.stack([row[:, None] * freqs, col[:, None] * freqs], axis=1).astype(np.float32)
    cos = np.cos(ang).reshape(SEQ, 32)
    sin = np.sin(ang).reshape(SEQ, 32)
    r = np.concatenate([cos, sin], axis=1).reshape(NLT, 128, 64).transpose(1, 0, 2).reshape(128, NLT * 64)
    return c, np.ascontiguousarray(r.astype(np.float32))


def make_in_maps(inp):
    f = lambda a: np.ascontiguousarray(np.asarray(a, dtype=np.float32))
    cst, rope = host_consts()
    c_ctx = f(inp["c_ctx"]).reshape(8, 128).T
    qkg = np.concatenate([np.tile(f(inp["q_norm_g"])[0], 8), np.tile(f(inp["k_norm_g"])[0], 2)])[None, :]
    aog = np.concatenate([f(inp["attn_out_g"])[0], f(inp["hgrn_out_g"])[0]]).reshape(8, 128).T
    lb = f(inp["hgrn_lb"])
    lbt = lb.reshape(2, 2, 4, 128).transpose(3, 0, 1, 2).reshape(128, 16)
    wr = np.concatenate([f(inp["w_router_grp"])[0], f(inp["w_router_exp"])[0]], axis=1)
    br = np.concatenate([f(inp["b_router_grp"])[0], f(inp["b_router_exp"])[0]])[None, :]
    weg = f(inp["w_exp_gate"])[0].reshape(32, 8, 128, 512).transpose(0, 2, 1, 3).reshape(4096, 8, 512)
    weu = f(inp["w_exp_up"])[0].reshape(32, 8, 128, 512).transpose(0, 2, 1, 3).reshape(4096, 8, 512)
    wed = f(inp["w_exp_down"])[0].reshape(32, 4, 128, 1024).transpose(0, 2, 1, 3).reshape(4096, 4, 1024)
    shared = dict(
        w_ada=f(inp["w_ada"])[0], b_ada=f(inp["b_ada"]), n1g=f(inp["norm1_g"]), n2g=f(inp["norm2_g"]),
        w_in=f(inp["w_in"])[0], qkg=f(qkg), aog=f(aog), lbt=f(lbt), w_out=f(inp["w_out"])[0],
        wr=f(wr), br=f(br), weg=f(weg), weu=f(weu), wed=f(wed), cst=cst, rope=rope)
    maps = []
    for b in range(NCORES):
        cv = np.concatenate([f(inp["c"])[b].reshape(8, 128).T, c_ctx], axis=1)
        m = dict(shared)
        m.update(x=f(inp["x"])[b], ctx=f(inp["ctx"])[b], cvec=f(cv))
        maps.append(m)
    return maps


def kernel(**inputs):
    nc, _ = build()
    maps = make_in_maps(inputs)
    res = run_bass_kernel_spmd(nc, maps, core_ids=list(range(NCORES)))
    out = np.stack([np.asarray(r["out"])[:SEQ] for r in res.results], axis=0)
    return out.astype(np.float32)
```
